# Optimizing a Trainium2 kernel written in Bass

```python
import jax, jax.numpy as jnp
from jax import lax
import numpy as np

D_MODEL = 1024
BATCH = 4
SEQ = 4096
DEPTH = 2

HEAD_DIM = 64
N_HEADS = 8
N_KV_HEADS = 2
ROT_DIM = HEAD_DIM // 4
ROPE_THETA = 500000.0
IDX_HEADS = 8
IDX_DIM = 64
MAX_TOPK = 256
Q_BLOCK = 128
POOL_CH = D_MODEL // 2
POOL_WINDOWS = (2, 4, 8, 16)
POOL_GROUP = POOL_CH // 4
N_EXPERTS = 32
TOP_K = 4
D_FF = D_MODEL
SWIGLU_LIMIT = 7.0
SWIGLU_ALPHA = 1.702
EXPERT_BLOCK = 128
PLE_DIM = 256
LN_EPS = 1e-5
DN_ALPHA = (2 * DEPTH) ** 0.25
DN_BETA = (8 * DEPTH) ** -0.25
SPLIT_SIZES = (N_HEADS * HEAD_DIM, N_KV_HEADS * HEAD_DIM, N_KV_HEADS * HEAD_DIM,
               IDX_HEADS * IDX_DIM, IDX_DIM, IDX_HEADS, POOL_CH, D_MODEL, D_MODEL)
D_IN = sum(SPLIT_SIZES)

kernel_name = 'hybrid_dsa_pool_moe_deepnorm'


def _layer_norm(x, g, b):
    xf = x.astype(jnp.float32)
    mu = xf.mean(-1, keepdims=True)
    var = jnp.square(xf - mu).mean(-1, keepdims=True)
    y = (xf - mu) * lax.rsqrt(var + LN_EPS) * g.astype(jnp.float32) + b.astype(jnp.float32)
    return y.astype(x.dtype)


def _rope_tables(L):
    pos = jnp.arange(L, dtype=jnp.float32)
    inv = ROPE_THETA ** (-jnp.arange(0, ROT_DIM, 2, dtype=jnp.float32) / ROT_DIM)
    ang = pos[:, None] * inv[None, :]
    return jnp.cos(ang), jnp.sin(ang)


def _partial_rope(x, cos, sin):
    half = ROT_DIM // 2
    xf = x.astype(jnp.float32)
    x1, x2 = xf[..., :half], xf[..., half:ROT_DIM]
    c, s = cos[None, :, None, :], sin[None, :, None, :]
    out = jnp.concatenate([x1 * c - x2 * s, x2 * c + x1 * s, xf[..., ROT_DIM:]], axis=-1)
    return out.astype(x.dtype)


def _dsa_attention(q, k, v, qi, ki, wi):
    B, L, H, Dh = q.shape
    n_sel = min(MAX_TOPK, L // 4)
    n_blk = L // Q_BLOCK
    group = N_HEADS // N_KV_HEADS
    key_pos = jnp.arange(L)
    wi = wi * (IDX_HEADS ** -0.5)

    def to_blocks(a):
        return a.reshape((B, n_blk, Q_BLOCK) + a.shape[2:]).swapaxes(0, 1)

    def one_block(args):
        qb, qib, wib, start = args
        qpos = start + jnp.arange(Q_BLOCK)
        causal = key_pos[None, :] <= qpos[:, None]
        dots = jnp.einsum('bqhd,bsd->bqhs', qib.astype(jnp.float32),
                          ki.astype(jnp.float32)) * (IDX_DIM ** -0.5)
        score = jnp.einsum('bqhs,bqh->bqs', jax.nn.relu(dots), wib.astype(jnp.float32))
        score = jnp.where(causal[None], score, -jnp.inf)
        _, sel = lax.top_k(score, n_sel)
        kg = jax.vmap(lambda kb, ib: kb[ib])(k, sel)
        vg = jax.vmap(lambda vb, ib: vb[ib])(v, sel)
        qg = qb.reshape(B, Q_BLOCK, N_KV_HEADS, group, Dh)
        logits = jnp.einsum('bqcgd,bqncd->bqcgn', qg, kg).astype(jnp.float32) * (Dh ** -0.5)
        valid = sel <= qpos[None, :, None]
        logits = jnp.where(valid[:, :, None, None, :], logits, -jnp.inf)
        probs = jax.nn.softmax(logits, axis=-1).astype(v.dtype)
        o = jnp.einsum('bqcgn,bqncd->bqcgd', probs, vg)
        return o.reshape(B, Q_BLOCK, H * Dh)

    starts = jnp.arange(n_blk) * Q_BLOCK
    out = lax.map(one_block, (to_blocks(q), to_blocks(qi), to_blocks(wi), starts))
    return out.swapaxes(0, 1).reshape(B, L, H * Dh)


def _pool_mixer(u, pool_w, pool_scale):
    B, L, C = u.shape
    uf = u.astype(jnp.float32)
    csum = jnp.pad(jnp.cumsum(uf, axis=1), ((0, 0), (1, 0), (0, 0)))
    pos1 = jnp.arange(1, L + 1, dtype=jnp.float32)[None, :, None]
    parts = []
    for g, win in enumerate(POOL_WINDOWS):
        sl = slice(g * POOL_GROUP, (g + 1) * POOL_GROUP)
        cg = csum[..., sl]
        upper = cg[:, 1:]
        lower = jnp.pad(cg, ((0, 0), (win - 1, 0), (0, 0)))[:, :L]
        mean = (upper - lower) / jnp.minimum(pos1, float(win))
        parts.append(mean - uf[..., sl])
    d = jnp.stack(parts, axis=2).astype(u.dtype)
    y = jnp.einsum('blgc,gcd->blgd', d, pool_w).reshape(B, L, C)
    return y * pool_scale


def _mixer(h, cos, sin, w_in, pool_w, pool_scale, w_up_attn, w_up_pool, w_out):
    B, L, _ = h.shape
    z = h @ w_in
    offsets = [int(o) for o in np.cumsum(SPLIT_SIZES)[:-1]]
    q, k, v, qi, ki, wi, u, g_attn, g_pool = jnp.split(z, offsets, axis=-1)
    q = _partial_rope(q.reshape(B, L, N_HEADS, HEAD_DIM), cos, sin)
    k = _partial_rope(k.reshape(B, L, N_KV_HEADS, HEAD_DIM), cos, sin)
    v = v.reshape(B, L, N_KV_HEADS, HEAD_DIM)
    qi = _partial_rope(qi.reshape(B, L, IDX_HEADS, IDX_DIM), cos, sin)
    ki = _partial_rope(ki.reshape(B, L, 1, IDX_DIM), cos, sin)[:, :, 0]
    y_attn = _dsa_attention(q, k, v, qi, ki, wi)
    y_pool = _pool_mixer(u, pool_w, pool_scale)
    merged = jax.nn.sigmoid(g_attn) * (y_attn @ w_up_attn) + jax.nn.sigmoid(g_pool) * (y_pool @ w_up_pool)
    return merged @ w_out


def _moe(h, router_w, router_b, w_gu, b_gu, w_down, b_down):
    B, L, D = h.shape
    N = B * L
    hf = h.reshape(N, D)
    logits = (hf @ router_w + router_b).astype(jnp.float32)
    top_val, top_idx = lax.top_k(logits, TOP_K)
    gates = jax.nn.softmax(top_val, axis=-1)
    A = N * TOP_K
    e_flat = top_idx.reshape(A).astype(jnp.int32)
    tok_flat = jnp.repeat(jnp.arange(N, dtype=jnp.int32), TOP_K)
    g_flat = gates.reshape(A)
    order = jnp.argsort(e_flat)
    e_sorted = e_flat[order]
    counts = jnp.zeros((N_EXPERTS,), jnp.int32).at[e_flat].add(1)
    padded = (counts + EXPERT_BLOCK - 1) // EXPERT_BLOCK * EXPERT_BLOCK
    pend = jnp.cumsum(padded)
    pstart = pend - padded
    gstart = jnp.cumsum(counts) - counts
    dest = pstart[e_sorted] + (jnp.arange(A, dtype=jnp.int32) - gstart[e_sorted])
    n_blocks = -(-A // EXPERT_BLOCK) + N_EXPERTS
    R = n_blocks * EXPERT_BLOCK
    row_tok = jnp.full((R,), N, jnp.int32).at[dest].set(tok_flat[order])
    row_gate = jnp.zeros((R,), jnp.float32).at[dest].set(g_flat[order])
    blk_start = jnp.arange(n_blocks, dtype=jnp.int32) * EXPERT_BLOCK
    blk_exp = jnp.minimum(jnp.searchsorted(pend, blk_start, side='right'), N_EXPERTS - 1)
    h_pad = jnp.concatenate([hf, jnp.zeros((1, D), hf.dtype)], axis=0)
    xr = h_pad[row_tok].reshape(n_blocks, EXPERT_BLOCK, D)

    def expert_block(args):
        xb, e = args
        gu = xb @ w_gu[e] + b_gu[e]
        gt, up = gu[:, :D_FF], gu[:, D_FF:]
        gt = jnp.minimum(gt, SWIGLU_LIMIT)
        up = jnp.clip(up, -SWIGLU_LIMIT, SWIGLU_LIMIT)
        act = gt * jax.nn.sigmoid(SWIGLU_ALPHA * gt) * (up + 1.0)
        return act @ w_down[e] + b_down[e]

    yr = lax.map(expert_block, (xr, blk_exp)).reshape(R, D)
    y = jax.ops.segment_sum(yr.astype(jnp.float32) * row_gate[:, None], row_tok, num_segments=N + 1)[:N]
    return y.astype(h.dtype).reshape(B, L, D)


def setup_inputs(seed: int = 0) -> dict:
    key = jax.random.key(seed)
    ks = jax.random.split(key, 24)

    def nrm(k, shape, scale):
        return jax.random.normal(k, shape, jnp.float32) * scale

    Ld = DEPTH
    return {
        'x': nrm(ks[0], (BATCH, SEQ, D_MODEL), 1.0),
        'p': nrm(ks[1], (DEPTH, BATCH, SEQ, PLE_DIM), 1.0),
        'ln0_g': 1.0 + nrm(ks[2], (D_MODEL,), 0.1),
        'ln0_b': nrm(ks[3], (D_MODEL,), 0.01),
        'w_in': nrm(ks[4], (Ld, D_MODEL, D_IN), D_MODEL ** -0.5),
        'pool_w': nrm(ks[5], (Ld, 4, POOL_GROUP, POOL_GROUP), POOL_GROUP ** -0.5),
        'pool_scale': 1.0 + nrm(ks[6], (Ld, POOL_CH), 0.1),
        'w_up_attn': nrm(ks[7], (Ld, N_HEADS * HEAD_DIM, D_MODEL), (N_HEADS * HEAD_DIM) ** -0.5),
        'w_up_pool': nrm(ks[8], (Ld, POOL_CH, D_MODEL), POOL_CH ** -0.5),
        'w_out': nrm(ks[9], (Ld, D_MODEL, D_MODEL), DN_BETA * D_MODEL ** -0.5),
        'ln1_g': 1.0 + nrm(ks[10], (Ld, D_MODEL), 0.1),
        'ln1_b': nrm(ks[11], (Ld, D_MODEL), 0.01),
        'router_w': nrm(ks[12], (Ld, D_MODEL, N_EXPERTS), D_MODEL ** -0.5),
        'router_b': nrm(ks[13], (Ld, N_EXPERTS), 0.01),
        'exp_w_gu': nrm(ks[14], (Ld, N_EXPERTS, D_MODEL, 2 * D_FF), D_MODEL ** -0.5),
        'exp_b_gu': nrm(ks[15], (Ld, N_EXPERTS, 2 * D_FF), 0.01),
        'exp_w_down': nrm(ks[16], (Ld, N_EXPERTS, D_FF, D_MODEL), DN_BETA * D_FF ** -0.5),
        'exp_b_down': nrm(ks[17], (Ld, N_EXPERTS, D_MODEL), 0.01),
        'ple_w_gate': nrm(ks[18], (Ld, D_MODEL, D_MODEL), D_MODEL ** -0.5),
        'ple_w_proj': nrm(ks[19], (Ld, PLE_DIM, D_MODEL), DN_BETA * PLE_DIM ** -0.5),
        'ln2_g': 1.0 + nrm(ks[20], (Ld, D_MODEL), 0.1),
        'ln2_b': nrm(ks[21], (Ld, D_MODEL), 0.01),
    }


def reference(x, p, ln0_g, ln0_b, w_in, pool_w, pool_scale, w_up_attn, w_up_pool, w_out,
              ln1_g, ln1_b, router_w, router_b, exp_w_gu, exp_b_gu, exp_w_down, exp_b_down,
              ple_w_gate, ple_w_proj, ln2_g, ln2_b):
    L = x.shape[1]
    cos, sin = _rope_tables(L)
    x = _layer_norm(x, ln0_g, ln0_b)
    for i in range(DEPTH):
        mix = _mixer(x, cos, sin, w_in[i], pool_w[i], pool_scale[i], w_up_attn[i], w_up_pool[i], w_out[i])
        x = _layer_norm(DN_ALPHA * x + mix, ln1_g[i], ln1_b[i])
        ffn = _moe(x, router_w[i], router_b[i], exp_w_gu[i], exp_b_gu[i], exp_w_down[i], exp_b_down[i])
        ple = jax.nn.sigmoid(x @ ple_w_gate[i]) * (p[i].astype(x.dtype) @ ple_w_proj[i])
        x = _layer_norm(DN_ALPHA * x + ffn + ple, ln2_g[i], ln2_b[i])
    return x
```

```python
from contextlib import ExitStack
import os
SKIP = set(os.environ.get('KSKIP', '').split(','))
import numpy as np
import concourse.bass as bass
import concourse.mybir as mybir
from concourse.bass_utils import run_bass_kernel_spmd

F32 = mybir.dt.float32
BF16 = mybir.dt.bfloat16
I32 = mybir.dt.int32
ALU = mybir.AluOpType
AF = mybir.ActivationFunctionType
AX = mybir.AxisListType

SAME_SYNC = True
NDMASEM = 12

D = 1024
NT = 16
CAP = 384
NEXP = 32
NIT = 16
ALPHA = 4.0 ** 0.25
EPS = 1e-5
IDX_SCALE = (64 ** -0.5) * (8 ** -0.5)
NEG = -1.0e30
MASKV = -240000.0


class Buf:
    __slots__ = ("name", "w", "r", "excl")

    def __init__(self, name="", excl=False):
        self.name = name
        self.w = None
        self.r = []
        self.excl = excl


class Stream:
    def __init__(self, name, eng):
        self.name = name
        self.eng = eng
        self.items = []
        self.waited = {}
        self.count = 0
        self.dma_i = 0


class Kern:
    def __init__(self, nc):
        self.nc = nc
        self.sems = {}
        self.sem_ctx = []
        self.streams = {}
        for name, eng in (("pe", nc.tensor), ("act", nc.scalar), ("dve", nc.vector),
                          ("pool", nc.gpsimd), ("sp", nc.sync)):
            self.streams[name] = Stream(name, eng)
        self.dma_cnt = {}
        self.nops = 0

    def sem(self, key):
        if key not in self.sems:
            cm = self.nc.semaphore("s_" + key)
            h = cm.__enter__()
            self.sem_ctx.append(cm)
            self.sems[key] = h
        return self.sems[key]

    def _wait(self, st, ev):
        if ev is None:
            return
        key, val = ev
        if st.waited.get(key, 0) >= val:
            return
        st.waited[key] = val
        sem = self.sem(key)
        st.items.append(lambda e, sem=sem, val=val: e.wait_ge(sem, val))

    @staticmethod
    def _deps(reads, writes):
        deps = []
        for b in reads:
            if b.w is not None:
                deps.append(b.w)
            if b.excl:
                deps.extend(b.r)
        for b in writes:
            if b.w is not None:
                deps.append(b.w)
            deps.extend(b.r)
        return deps

    @staticmethod
    def _mark(ev, reads, writes):
        for b in reads:
            if b.excl:
                b.w = ev
                b.r = []
            else:
                b.r.append(ev)
        for b in writes:
            b.w = ev
            b.r = []

    def op(self, eng, fn, reads=(), writes=()):
        st = self.streams[eng]
        own = "e_" + eng
        for ev in self._deps(reads, writes):
            if ev[0] == own and (eng == "pe" or not SAME_SYNC):
                continue
            self._wait(st, ev)
        st.count += 1
        ev = (own, st.count)
        sem = self.sem(own)
        st.items.append(lambda e, fn=fn, sem=sem: fn(e).then_inc(sem, 1))
        self._mark(ev, reads, writes)
        self.nops += 1
        return ev

    def dma(self, q, fn, reads=(), writes=()):
        st = self.streams[q]
        slot = st.dma_i % NDMASEM
        st.dma_i += 1
        key = "d_%s_%d" % (q, slot)
        prev = self.dma_cnt.get(key, 0)
        if prev:
            self._wait(st, (key, prev))
        for ev in self._deps(reads, writes):
            self._wait(st, ev)
        val = prev + 16
        self.dma_cnt[key] = val
        ev = (key, val)
        sem = self.sem(key)
        st.items.append(lambda e, fn=fn, sem=sem: fn(e).then_inc(sem, 16))
        self._mark(ev, reads, writes)
        self.nops += 1
        return ev

    def coll(self, fn, reads=(), writes=()):
        st = self.streams["pool"]
        key = "coll"
        prev = self.dma_cnt.get(key, 0)
        if prev:
            self._wait(st, (key, prev))
        for ev in self._deps(reads, writes):
            self._wait(st, ev)
        val = prev + 16
        self.dma_cnt[key] = val
        ev = (key, val)
        sem = self.sem(key)
        st.items.append(lambda e, fn=fn, sem=sem: fn(e).then_inc(sem, 16))
        self._mark(ev, reads, writes)
        return ev

    def barrier(self):
        for st in self.streams.values():
            for o in self.streams.values():
                if o.count:
                    self._wait(st, ("e_" + o.name, o.count))
            for key, val in self.dma_cnt.items():
                self._wait(st, (key, val))

    def emit(self):
        nc = self.nc
        with nc.Block() as block:
            def mk(st):
                def body(e):
                    for it in st.items:
                        it(e)
                return body
            block.tensor(mk(self.streams["pe"]))
            block.scalar(mk(self.streams["act"]))
            block.vector(mk(self.streams["dve"]))
            block.gpsimd(mk(self.streams["pool"]))
            block.sync(mk(self.streams["sp"]))

    def close(self):
        for cm in reversed(self.sem_ctx):
            cm.__exit__(None, None, None)


class T:
    def __init__(self, t, name, excl=False):
        self.t = t
        self.b = Buf(name, excl)

    def __getitem__(self, k):
        return self.t[k]


def build(layers=(0, 1), nq=NT, dbg=()):
    stop_after = "D"
    nc = bass.Bass("TRN2", target_bir_lowering=False)
    K = Kern(nc)

    def din(name, shape, dt=F32):
        return nc.dram_tensor(name, list(shape), dt, kind="ExternalInput").ap()

    def dout(name, shape, dt=F32):
        return nc.dram_tensor(name, list(shape), dt, kind="ExternalOutput").ap()

    xown = din("xown", [NT, 128, D])
    xoth = din("xoth", [NT, 128, D])
    pown_all = din("pown", [2, NT, 128, 256])
    cs_own = din("cs_own", [NT, 128, 128])
    cs_oth = din("cs_oth", [NT, 128, 128])
    dmask = din("dmask", [128, 256])
    hmix = din("hmix", [128, 2])
    pdiv0 = din("pdiv0", [128, 64])
    poth0 = din("poth0", [NT, 128, 256])
    dmask_p = din("dmask_p", [128, 256]); hmix_p = din("hmix_p", [128, 2]); pdiv0_p = din("pdiv0_p", [128, 64])
    ROLE = {False: dict(xown=xown, xoth=xoth, cs_own=cs_own, cs_oth=cs_oth, dmask=dmask, hmix=hmix, pdiv0=pdiv0),
            True: dict(xown=xoth, xoth=xown, cs_own=cs_oth, cs_oth=cs_own, dmask=dmask_p, hmix=hmix_p, pdiv0=pdiv0_p)}
    AL = {}
    for nm, shp in (("w_in", [D, 3912]), ("pool_w", [4, 128, 128]), ("pool_scale", [512]), ("w_up_attn", [512, D]),
                    ("w_up_pool", [512, D]), ("w_out", [D, D]), ("ln1_g", [D]), ("ln1_b", [D]), ("router_w", [D, 32]),
                    ("router_b", [32]), ("exp_w_gu", [NEXP, D, 2048]), ("exp_b_gu", [NEXP, 2048]),
                    ("exp_w_down", [NEXP, D, D]), ("exp_b_down", [NEXP, D]), ("ple_w_gate", [D, D]),
                    ("ple_w_proj", [256, D]), ("ln2_g", [D]), ("ln2_b", [D])):
        if nm in ("exp_w_gu", "exp_w_down"):
            AL[nm] = [din("%s_%d" % (nm, i), shp) for i in range(2)]
        else:
            AL[nm] = din(nm, [2] + shp)
    ln0_g = din("ln0_g", [D]); ln0_b = din("ln0_b", [D])
    yout = dout("yout", [NT, 128, D])
    dbg_out = {}
    xo1 = nc.dram_tensor("xo1", [NT * 128, D], BF16).ap()
    bxo1 = Buf("xo1")
    Xs = nc.dram_tensor("Xs", [NEXP * CAP + 512, D], BF16).ap()
    Ys = nc.dram_tensor("Ys", [NEXP * CAP, D], F32).ap()
    bXs = Buf("Xs"); bYs = Buf("Ys")

    top = ExitStack()

    uniq = [0]

    def sb(es, name, shape, dt=F32):
        uniq[0] += 1
        name = "%s_%d" % (name, uniq[0])
        return T(es.enter_context(nc.sbuf_tensor(name, list(shape), dt)), name)

    def ps(es, name, shape, dt=F32):
        return T(es.enter_context(nc.psum_tensor(name, list(shape), dt)), name, excl=True)

    def bcast_row(vec_ap, n):
        return vec_ap.partition_broadcast(128)

    PB = [ps(top, "pb%d" % i, [128, 512]) for i in range(7)]
    PT = ps(top, "pt", [128, 8, 128], BF16)

    iot = sb(top, "iot", [128, 128]); pidx = sb(top, "pidx", [128, 1])
    identf = sb(top, "identf", [128, 128]); identb = sb(top, "identb", [128, 128], BF16)
    ones_b = sb(top, "ones_b", [128, 128], BF16)
    K.op("pool", lambda e: e.iota(iot[:], pattern=[[1, 128]], base=0, channel_multiplier=0,
                                  allow_small_or_imprecise_dtypes=True), writes=[iot.b])
    K.op("pool", lambda e: e.iota(pidx[:], pattern=[[0, 1]], base=0, channel_multiplier=1,
                                  allow_small_or_imprecise_dtypes=True), writes=[pidx.b])
    K.op("dve", lambda e: e.tensor_scalar(out=identf[:], in0=iot[:], scalar1=pidx[:, 0:1], scalar2=None,
                                          op0=ALU.is_equal), reads=[iot.b, pidx.b], writes=[identf.b])
    K.op("dve", lambda e: e.tensor_copy(out=identb[:], in_=identf[:]), reads=[identf.b], writes=[identb.b])
    K.op("dve", lambda e: e.memset(ones_b[:], 1.0), writes=[ones_b.b])

    xres = [sb(top, "xres%d" % j, [128, D]) for j in range(NT)]
    gvec = sb(top, "gvec", [128, D]); bvec = sb(top, "bvec", [128, D])

    def load_ln(g_ap, b_ap):
        K.dma("sp", lambda e: e.dma_start(out=gvec[:], in_=g_ap.partition_broadcast(128)), writes=[gvec.b])
        K.dma("sp", lambda e: e.dma_start(out=bvec[:], in_=b_ap.partition_broadcast(128)), writes=[bvec.b])

    lnst = sb(top, "lnst", [128, 2, 6]); lnmv = sb(top, "lnmv", [128, 2]); lnr = sb(top, "lnr", [128, 1])

    def layer_norm(xt):
        for h in range(2):
            K.op("dve", lambda e, h=h: e.bn_stats(out=lnst[:, h, :], in_=xt[:, h * 512:(h + 1) * 512]),
                 reads=[xt.b], writes=[lnst.b])
        K.op("dve", lambda e: e.bn_aggr(out=lnmv[:], in_=lnst[:]), reads=[lnst.b], writes=[lnmv.b])
        K.op("dve", lambda e: e.tensor_scalar(out=lnr[:], in0=lnmv[:, 1:2], scalar1=EPS, scalar2=None,
                                              op0=ALU.add), reads=[lnmv.b], writes=[lnr.b])
        K.op("act", lambda e: e.activation(out=lnr[:], in_=lnr[:], func=AF.Sqrt), reads=[lnr.b], writes=[lnr.b])
        K.op("dve", lambda e: e.reciprocal(out=lnr[:], in_=lnr[:]), reads=[lnr.b], writes=[lnr.b])
        K.op("dve", lambda e: e.tensor_scalar(out=xt[:], in0=xt[:], scalar1=lnmv[:, 0:1], scalar2=lnr[:, 0:1],
                                              op0=ALU.subtract, op1=ALU.mult), reads=[xt.b, lnmv.b, lnr.b], writes=[xt.b])
        K.op("dve", lambda e: e.tensor_tensor(out=xt[:], in0=xt[:], in1=gvec[:], op=ALU.mult),
             reads=[xt.b, gvec.b], writes=[xt.b])
        K.op("dve", lambda e: e.tensor_tensor(out=xt[:], in0=xt[:], in1=bvec[:], op=ALU.add),
             reads=[xt.b, bvec.b], writes=[xt.b])

    def transpose_x(xt, xT, xTf=None):
        for half in range(2):
            pb = PB[half]
            for k in range(4):
                kk = half * 4 + k
                K.op("pe", lambda e, pb=pb, k=k, kk=kk: e.transpose(out=pb[:, k * 128:(k + 1) * 128],
                                                                    in_=xt[:, kk * 128:(kk + 1) * 128], identity=identf[:]),
                     reads=[xt.b, identf.b], writes=[pb.b])
            K.op("act", lambda e, pb=pb, half=half: e.activation(
                out=xT[:, half * 4:(half + 1) * 4, :], in_=pb[:].rearrange("p (k t) -> p k t", k=4), func=AF.Copy),
                reads=[pb.b], writes=[xT.b])
            if xTf is not None:
                K.op("dve", lambda e, pb=pb, half=half: e.tensor_copy(
                    out=xTf[:, half * 4:(half + 1) * 4, :], in_=pb[:].rearrange("p (k t) -> p k t", k=4)),
                    reads=[pb.b], writes=[xTf.b])

    def rope(src, dst, cst, nh):
        sv, dv, sbuf, dbuf = src
        cos = cst[:, 0, 0:nh, :]
        sin = cst[:, 1, 0:nh, :]
        x1 = sv[:, :, 0:8]; x2 = sv[:, :, 8:16]
        if 'rope_act' not in SKIP: K.op("act", lambda e: e.activation(out=dv, in_=sv, func=AF.Copy), reads=[sbuf], writes=[dbuf])
        t = dst
        if 'rope_dve' in SKIP: return
        K.op("dve", lambda e: e.tensor_tensor(out=t[:, 0, 0:nh, :], in0=x1, in1=cos, op=ALU.mult), reads=[sbuf, cst.b], writes=[t.b])
        K.op("dve", lambda e: e.tensor_tensor(out=t[:, 1, 0:nh, :], in0=x2, in1=sin, op=ALU.mult), reads=[sbuf, cst.b], writes=[t.b])
        K.op("dve", lambda e: e.tensor_tensor(out=t[:, 2, 0:nh, :], in0=x2, in1=cos, op=ALU.mult), reads=[sbuf, cst.b], writes=[t.b])
        K.op("dve", lambda e: e.tensor_tensor(out=t[:, 3, 0:nh, :], in0=x1, in1=sin, op=ALU.mult), reads=[sbuf, cst.b], writes=[t.b])
        K.op("dve", lambda e: e.tensor_tensor(out=dv[:, :, 0:8], in0=t[:, 0, 0:nh, :], in1=t[:, 1, 0:nh, :], op=ALU.subtract),
             reads=[t.b], writes=[dbuf])
        K.op("dve", lambda e: e.tensor_tensor(out=dv[:, :, 8:16], in0=t[:, 2, 0:nh, :], in1=t[:, 3, 0:nh, :], op=ALU.add),
             reads=[t.b], writes=[dbuf])

    ropet = sb(top, "ropet", [128, 4, 8, 8])

    slots = sb(top, "slots", [128, NT, 4], I32); gates = sb(top, "gates", [128, NT, 4])
    class V:
        def __init__(self, ap, b):
            self.ap = ap; self.b = b

        def __getitem__(self, k):
            return self.ap[k]

    def run_layer(l, partner=False, zero_slabs=False):
        first = (l == 0)
        fused_in = (l == 1)
        last = (l == 1)
        R = ROLE[partner]
        xown = R["xown"]; xoth = R["xoth"]; cs_own = R["cs_own"]; cs_oth = R["cs_oth"]
        dmask = R["dmask"]; hmix = R["hmix"]; pdiv0 = R["pdiv0"]
        pown = poth0 if partner else pown_all[l]
        w_in = AL["w_in"][l]; pool_w = AL["pool_w"][l]; pool_scale = AL["pool_scale"][l]
        w_up_attn = AL["w_up_attn"][l]; w_up_pool = AL["w_up_pool"][l]; w_out = AL["w_out"][l]
        ln1_g = AL["ln1_g"][l]; ln1_b = AL["ln1_b"][l]; router_w = AL["router_w"][l]; router_b = AL["router_b"][l]
        exp_w_gu = AL["exp_w_gu"][l]; exp_b_gu = AL["exp_b_gu"][l]; exp_w_down = AL["exp_w_down"][l]; exp_b_down = AL["exp_b_down"][l]
        ple_w_gate = AL["ple_w_gate"][l]; ple_w_proj = AL["ple_w_proj"][l]; ln2_g = AL["ln2_g"][l]; ln2_b = AL["ln2_b"][l]
        esY = ExitStack()
        yaT = sb(esY, "yaT", [128, 4, NT * 128], BF16)
        uh = sb(esY, "uh", [128, NT, 4, 16])
        esAB = ExitStack()
        kT = sb(esAB, "kT", [128, 2, 2 * NT * 128], BF16)
        kiT = sb(esAB, "kiT", [128, 2 * NT * 128], BF16)
        K.op("dve", lambda e: e.memset(kT[64:128, :, :], 0.0), writes=[kT.b])
        K.op("dve", lambda e: e.memset(kiT[64:128, :], 0.0), writes=[kiT.b])
        vaug = sb(esAB, "vaug", [128, 2 * NT, 2, 65], BF16)
        K.op("dve", lambda e: e.memset(vaug[:], 1.0), writes=[vaug.b])

        if first:
            load_ln(ln0_g, ln0_b)

        esA = ExitStack()
        Wkv = sb(esA, "Wkv", [128, 8, 320], BF16)
        Wu_a = sb(esA, "Wu_a", [128, 8, 512], BF16)
        xtmp = sb(esA, "xtmp", [128, D])
        xTa = [sb(esA, "xTa%d" % i, [128, 8, 128], BF16) for i in range(2)]
        kvb = sb(esA, "kvb", [128, 5, 64], BF16)
        csa = sb(esA, "csa", [128, 2, 8, 8])
        if fused_in:
            xbm = sb(esA, "xbm", [128, D], BF16)

        def wslice(a, b):
            return w_in[:, a:b].rearrange("(k p) n -> p k n", p=128)

        zt = sb(esA, "zt", [128, 8, D], BF16)
        K.op("dve", lambda e: e.memset(zt[:], 0.0), writes=[zt.b])
        nrows = NEXP * CAP + 512
        for r0 in (range(0, nrows, 1024) if zero_slabs else ()):
            nb_ = min(1024, nrows - r0) // 128
            K.dma("sp", lambda e, r0=r0, nb_=nb_: e.dma_start(out=Xs[r0:r0 + nb_ * 128, :].rearrange("(a p) n -> p a n", p=128), in_=zt[:, 0:nb_, :]),
                  reads=[zt.b], writes=[bXs])

        K.dma("pool", lambda e: e.dma_start(out=Wkv[:, :, 0:128], in_=wslice(512, 640)), writes=[Wkv.b])
        K.dma("pool", lambda e: e.dma_start(out=Wkv[:, :, 128:192], in_=wslice(1280, 1344)), writes=[Wkv.b])
        K.dma("pool", lambda e: e.dma_start(out=Wkv[:, :, 192:320], in_=wslice(640, 768)), writes=[Wkv.b])
        K.dma("pool", lambda e: e.dma_start(out=Wu_a[:], in_=wslice(1352, 1864)), writes=[Wu_a.b])

        it = 0
        for j in range(nq):
            for s in range(2):
                kt = 2 * j + s
                src = xown if s == 0 else xoth
                cs_src = cs_own if s == 0 else cs_oth
                xt = xres[j] if s == 0 else xtmp
                xT = xTa[it % 2]; it += 1
                K.dma("sp", lambda e, cs_src=cs_src, j=j: e.dma_start(out=csa[:].rearrange("p a h d -> p (a h d)"), in_=cs_src[j]),
                      writes=[csa.b])
                if not fused_in:
                    K.dma("sp", lambda e, xt=xt, src=src, j=j: e.dma_start(out=xt[:], in_=src[j]), writes=[xt.b])
                    if first:
                        layer_norm(xt)
                    transpose_x(xt, xT)
                elif s == 0:
                    transpose_x(xt, xT)
                else:
                    K.dma("sp", lambda e, j=j: e.dma_start(out=xbm[:], in_=xo1[j * 128:(j + 1) * 128, :]), reads=[bxo1], writes=[xbm.b])
                    for k in range(8):
                        K.op("pe", lambda e, k=k: e.transpose(out=PT[:, k, :], in_=xbm[:, k * 128:(k + 1) * 128], identity=identb[:]),
                             reads=[xbm.b, identb.b], writes=[PT.b])
                    K.op("act", lambda e, xT=xT: e.activation(out=xT[:], in_=PT[:], func=AF.Copy), reads=[PT.b], writes=[xT.b])
                pb = PB[2]
                for k in range(8):
                    K.op("pe", lambda e, pb=pb, xT=xT, k=k: e.matmul(pb[:, 0:320], lhsT=xT[:, k, :], rhs=Wkv[:, k, :],
                                                                     start=(k == 0), stop=(k == 7)),
                         reads=[xT.b, Wkv.b], writes=[pb.b])
                if s == 1 and 'uh' not in SKIP:
                    for g in range(4):
                        for k in range(8):
                            K.op("pe", lambda e, pb=pb, xT=xT, k=k, g=g: e.matmul(
                                pb[:, 384 + g * 16:384 + (g + 1) * 16], lhsT=Wu_a[:, k, g * 128:(g + 1) * 128],
                                rhs=xT[:, k, 112:128], start=(k == 0), stop=(k == 7)),
                                reads=[xT.b, Wu_a.b], writes=[pb.b])
                    K.op("act", lambda e, pb=pb, j=j: e.activation(out=uh[:, j, :, :], in_=pb[:, 384:448].rearrange("p (g t) -> p g t", g=4),
                                                                   func=AF.Copy), reads=[pb.b], writes=[uh.b])
                if 'vaug' not in SKIP: K.op("act", lambda e, pb=pb, kt=kt: e.activation(out=vaug[:, kt, :, 0:64],
                                                                in_=pb[:, 192:320].rearrange("p (c d) -> p c d", c=2), func=AF.Copy),
                     reads=[pb.b], writes=[vaug.b])
                if 'rope' not in SKIP: rope((pb[:, 0:192].rearrange("p (h d) -> p h d", h=3), kvb[:, 0:3, :], pb.b, kvb.b), ropet, csa, 3)
                for h in range(3 if 'ktr' not in SKIP else 0):
                    K.op("pe", lambda e, h=h: e.transpose(out=PT[0:64, h, :], in_=kvb[:, h, :], identity=identb[:]),
                         reads=[kvb.b, identb.b], writes=[PT.b])
                K.op("act", lambda e, kt=kt: e.activation(out=kT[0:64, :, kt * 128:(kt + 1) * 128], in_=PT[0:64, 0:2, :], func=AF.Copy),
                     reads=[PT.b], writes=[kT.b])
                K.op("dve", lambda e, kt=kt: e.tensor_copy(out=kiT[0:64, kt * 128:(kt + 1) * 128], in_=PT[0:64, 2, :]),
                     reads=[PT.b], writes=[kiT.b])
        K.barrier()
        if stop_after == "A":
            dk = dout("d_kT", [64, 2 * NT * 128], BF16)
            K.dma("sp", lambda e: e.dma_start(out=dk, in_=kiT[:]), reads=[kiT.b])
            K.barrier()
            K.emit()
            K.close()
            return nc
        esA.close()

        esB = ExitStack()
        Wqq = sb(esB, "Wqq", [128, 8, 1032], BF16)
        K.dma("pool", lambda e: e.dma_start(out=Wqq[:, :, 0:512], in_=wslice(0, 512)), writes=[Wqq.b])
        K.dma("pool", lambda e: e.dma_start(out=Wqq[:, :, 512:1024], in_=wslice(768, 1280)), writes=[Wqq.b])
        K.dma("pool", lambda e: e.dma_start(out=Wqq[:, :, 1024:1032], in_=wslice(1344, 1352)), writes=[Wqq.b])
        order = []
        lo_, hi_ = 0, nq - 1
        while lo_ <= hi_:
            order.append(hi_); hi_ -= 1
            if lo_ <= hi_:
                order.append(lo_); lo_ += 1
        nks = [(2 * j_ + 2) * 128 for j_ in order]
        WS = max([nks[0]] + [nks[i] + nks[i + 1] for i in range(len(nks) - 1)])
        score_t = sb(esB, "score", [128, WS])
        sbufs = [Buf("scoreA"), Buf("scoreB")]
        mb_t = sb(esB, "mb", [128, WS], BF16)
        mbufs = [Buf("mbA"), Buf("mbB")]
        mbT = sb(esB, "mbT", [128, 2 * NT, 128], BF16)
        xTq = sb(esB, "xTq", [128, 8, 128], BF16)
        qb = sb(esB, "qb", [128, 8, 64], BF16); qib = sb(esB, "qib", [128, 8, 64], BF16)
        qTs = [sb(esB, "qT%d" % i, [128, 8, 128], BF16) for i in range(3)]; qiT = sb(esB, "qiT", [128, 8, 128], BF16)
        for t_ in (qTs[0], qTs[1], qTs[2], qiT):
            K.op("dve", lambda e, t_=t_: e.memset(t_[64:128, :, :], 0.0), writes=[t_.b])
        csq = sb(esB, "csq", [128, 2, 8, 8])
        wif = sb(esB, "wif", [128, 8]); absw = sb(esB, "absw", [128, 8]); sgn = sb(esB, "sgn", [128, 8])
        Dg = sb(esB, "Dg", [128, 8, 128], BF16)
        rbuf = [sb(esB, "rbuf%d" % i, [128, 512], BF16) for i in range(3)]
        PTs = [sb(esB, "PTs%d" % i, [128, 4, 128], BF16) for i in range(2)]
        dmk = sb(esB, "dmk", [128, 256])
        bmins = [sb(esB, "bmin%d" % i, [128, 8]) for i in range(2)]
        lo0s = [sb(esB, "lo0%d" % i, [128, 1]) for i in range(2)]; hi0s = [sb(esB, "hi0%d" % i, [128, 1]) for i in range(2)]
        wtab = sb(esB, "wtab", [128, NIT + 1]); p2 = sb(esB, "p2", [128, NIT + 1])
        mid = sb(esB, "mid", [128, 1]); cnt = sb(esB, "cnt", [128, 1]); tt = sb(esB, "tt", [128, 1])
        rng = sb(esB, "rng", [128, 1]); thr = sb(esB, "thr", [128, 1])
        rec = sb(esB, "rec", [128, 8]); yb = sb(esB, "yb", [128, 8, 64], BF16)
        K.dma("sp", lambda e: e.dma_start(out=dmk[:], in_=dmask), writes=[dmk.b])
        for n in range(NIT + 1):
            K.op("dve", lambda e, n=n: e.memset(p2[:, n:n + 1], 0.5 ** (n + 1)), writes=[p2.b])

        def stA(p_):
            j = order[p_]
            nkt = 2 * j + 2
            nk = nkt * 128
            off_ = 0 if p_ % 2 == 0 else WS - nk
            qT = qTs[p_ % 3]; bmin = bmins[p_ % 2]; lo0 = lo0s[p_ % 2]; hi0 = hi0s[p_ % 2]
            score = V(score_t[:, off_:off_ + nk], sbufs[p_ % 2]); mb = V(mb_t[:, off_:off_ + nk], mbufs[p_ % 2])
            K.dma("sp", lambda e, j=j: e.dma_start(out=csq[:].rearrange("p a h d -> p (a h d)"), in_=cs_own[j]), writes=[csq.b])
            transpose_x(xres[j], xTq)
            for blk, pb in ((0, PB[2]), (1, PB[3])):
                for k in range(8):
                    K.op("pe", lambda e, pb=pb, k=k, blk=blk: e.matmul(pb[:], lhsT=xTq[:, k, :], rhs=Wqq[:, k, blk * 512:(blk + 1) * 512],
                                                                       start=(k == 0), stop=(k == 7)),
                         reads=[xTq.b, Wqq.b], writes=[pb.b])
            for k in range(8):
                K.op("pe", lambda e, k=k: e.matmul(PB[4][:, 0:8], lhsT=xTq[:, k, :], rhs=Wqq[:, k, 1024:1032],
                                                   start=(k == 0), stop=(k == 7)),
                     reads=[xTq.b, Wqq.b], writes=[PB[4].b])
            rope((PB[2][:].rearrange("p (h d) -> p h d", h=8), qb[:], PB[2].b, qb.b), ropet, csq, 8)
            rope((PB[3][:].rearrange("p (h d) -> p h d", h=8), qib[:], PB[3].b, qib.b), ropet, csq, 8)
            K.op("act", lambda e: e.activation(out=wif[:], in_=PB[4][:, 0:8], func=AF.Copy), reads=[PB[4].b], writes=[wif.b])
            K.op("act", lambda e: e.activation(out=absw[:], in_=wif[:], func=AF.Abs, scale=IDX_SCALE), reads=[wif.b], writes=[absw.b])
            K.op("dve", lambda e: e.tensor_scalar(out=sgn[:], in0=wif[:], scalar1=0.0, scalar2=2.0,
                                                  op0=ALU.is_ge, op1=ALU.mult), reads=[wif.b], writes=[sgn.b])
            K.op("dve", lambda e: e.tensor_scalar(out=sgn[:], in0=sgn[:], scalar1=-1.0, scalar2=None,
                                                  op0=ALU.add), reads=[sgn.b], writes=[sgn.b])
            for h in range(8):
                K.op("dve", lambda e, h=h: e.tensor_scalar(out=Dg[:, h, :], in0=identb[:], scalar1=sgn[:, h:h + 1], scalar2=None,
                                                           op0=ALU.mult), reads=[identb.b, sgn.b], writes=[Dg.b])
            for h in range(8):
                K.op("pe", lambda e, h=h: e.transpose(out=PT[0:64, h, :], in_=qb[:, h, :], identity=identb[:]),
                     reads=[qb.b, identb.b], writes=[PT.b])
            K.op("act", lambda e: e.activation(out=qT[0:64, :, :], in_=PT[0:64, :, :], func=AF.Copy), reads=[PT.b], writes=[qT.b])
            for h in range(8):
                K.op("pe", lambda e, h=h: e.transpose(out=PT[0:64, h, :], in_=qib[:, h, :], identity=identb[:]),
                     reads=[qib.b, identb.b], writes=[PT.b])
            K.op("act", lambda e: e.activation(out=qiT[0:64, :, :], in_=PT[0:64, :, :], func=AF.Copy), reads=[PT.b], writes=[qiT.b])

            nblk = (nk + 511) // 512
            steps = [(blk, h) for blk in range(nblk) for h in range(8)]

            def dots(i):
                blk, h = steps[i]
                c0 = blk * 512; w = min(512, nk - c0)
                pd = PB[5 + (i % 2)]
                K.op("pe", lambda e: e.matmul(pd[:, 0:w], lhsT=qiT[:, h, :], rhs=kiT[:, c0:c0 + w], start=True, stop=True),
                     reads=[qiT.b, kiT.b], writes=[pd.b])

            def relu_acc(i):
                blk, h = steps[i]
                c0 = blk * 512; w = min(512, nk - c0)
                pd = PB[5 + (i % 2)]; rb = rbuf[i % 3]
                if h % 3 != 2:
                    K.op("act", lambda e: e.activation(out=rb[:, 0:w], in_=pd[:, 0:w], func=AF.Relu, scale=absw[:, h:h + 1]),
                         reads=[pd.b, absw.b], writes=[rb.b])
                else:
                    K.op("dve", lambda e: e.tensor_scalar(out=rb[:, 0:w], in0=pd[:, 0:w], scalar1=absw[:, h:h + 1], scalar2=0.0,
                                                          op0=ALU.mult, op1=ALU.max), reads=[pd.b, absw.b], writes=[rb.b])
                K.op("pe", lambda e: e.matmul(PB[4][:, 0:w], lhsT=Dg[:, h, :], rhs=rb[:, 0:w], start=(h == 0), stop=(h == 7)),
                     reads=[Dg.b, rb.b], writes=[PB[4].b])
                if h == 7:
                    K.op("dve", lambda e: e.tensor_scalar(out=score[:, c0:c0 + w], in0=PB[4][:, 0:w], scalar1=0.0, scalar2=3.0e38,
                                                          op0=ALU.add, op1=ALU.min, accum_out=bmin[:, blk:blk + 1]),
                         reads=[PB[4].b], writes=[score.b, bmin.b])

            for i in range(len(steps) + 1):
                if i < len(steps):
                    dots(i)
                if i >= 1:
                    relu_acc(i - 1)
            K.op("dve", lambda e, nblk=nblk: e.tensor_reduce(out=lo0[:], in_=bmin[:, 0:nblk], axis=AX.X, op=ALU.min),
                 reads=[bmin.b], writes=[lo0.b])
            K.op("dve", lambda e, nk=nk: e.tensor_tensor(out=score[:, nk - 256:nk], in0=score[:, nk - 256:nk], in1=dmk[:], op=ALU.add),
                 reads=[score.b, dmk.b], writes=[score.b])
            K.op("dve", lambda e, nk=nk: e.tensor_reduce(out=hi0[:], in_=score[:, 0:nk], axis=AX.X, op=ALU.max),
                 reads=[score.b], writes=[hi0.b])

        def stB(p_):
            j = order[p_]
            nkt = 2 * j + 2
            nk = nkt * 128
            off_ = 0 if p_ % 2 == 0 else WS - nk
            qT = qTs[p_ % 3]; bmin = bmins[p_ % 2]; lo0 = lo0s[p_ % 2]; hi0 = hi0s[p_ % 2]
            score = V(score_t[:, off_:off_ + nk], sbufs[p_ % 2]); mb = V(mb_t[:, off_:off_ + nk], mbufs[p_ % 2])
            K.op("dve", lambda e: e.tensor_tensor(out=rng[:], in0=hi0[:], in1=lo0[:], op=ALU.subtract), reads=[hi0.b, lo0.b], writes=[rng.b])
            K.op("dve", lambda e: e.tensor_scalar(out=rng[:], in0=rng[:], scalar1=1.0001, scalar2=1e-20, op0=ALU.mult, op1=ALU.add),
                 reads=[rng.b], writes=[rng.b])
            K.op("dve", lambda e: e.tensor_scalar(out=wtab[:], in0=p2[:], scalar1=rng[:, 0:1], scalar2=None, op0=ALU.mult),
                 reads=[p2.b, rng.b], writes=[wtab.b])
            K.op("dve", lambda e: e.tensor_tensor(out=mid[:], in0=lo0[:], in1=wtab[:, 0:1], op=ALU.add), reads=[lo0.b, wtab.b], writes=[mid.b])
            for n in range(NIT):
                K.op("dve", lambda e, nk=nk: e.tensor_scalar(out=mb[:, 0:nk], in0=score[:, 0:nk], scalar1=mid[:, 0:1], scalar2=0.0,
                                                             op0=ALU.is_ge, op1=ALU.add, accum_out=cnt[:]),
                     reads=[score.b, mid.b], writes=[mb.b, cnt.b])
                K.op("dve", lambda e: e.tensor_scalar(out=tt[:], in0=cnt[:], scalar1=255.5, scalar2=0.5, op0=ALU.is_ge, op1=ALU.subtract),
                     reads=[cnt.b], writes=[tt.b])
                K.op("dve", lambda e, n=n: e.scalar_tensor_tensor(out=mid[:], in0=tt[:], scalar=wtab[:, n:n + 1], in1=mid[:],
                                                                  op0=ALU.mult, op1=ALU.add),
                     reads=[tt.b, wtab.b, mid.b], writes=[mid.b])
            K.op("dve", lambda e: e.tensor_tensor(out=thr[:], in0=mid[:], in1=wtab[:, NIT:NIT + 1], op=ALU.subtract),
                 reads=[mid.b, wtab.b], writes=[thr.b])
            if "score" in dbg:
                K.dma("sp", lambda e, j=j, nk=nk: e.dma_start(out=dbg_out["score"][j][:, 0:nk], in_=score[:, 0:nk]), reads=[score.b])
                K.dma("sp", lambda e, j=j: e.dma_start(out=dbg_out["thr"][j][:, 0:1], in_=thr[:], allow_slow_non_contiguous=True), reads=[thr.b])
                K.dma("sp", lambda e, j=j: e.dma_start(out=dbg_out["thr"][j][:, 1:2], in_=cnt[:], allow_slow_non_contiguous=True), reads=[cnt.b])
            K.op("dve", lambda e, nk=nk: e.tensor_scalar(out=mb[:, 0:nk], in0=score[:, 0:nk], scalar1=thr[:, 0:1], scalar2=MASKV,
                                                         op0=ALU.is_lt, op1=ALU.mult), reads=[score.b, thr.b], writes=[mb.b])

        def stC(p_):
            j = order[p_]
            nkt = 2 * j + 2
            nk = nkt * 128
            off_ = 0 if p_ % 2 == 0 else WS - nk
            qT = qTs[p_ % 3]; bmin = bmins[p_ % 2]; lo0 = lo0s[p_ % 2]; hi0 = hi0s[p_ % 2]
            score = V(score_t[:, off_:off_ + nk], sbufs[p_ % 2]); mb = V(mb_t[:, off_:off_ + nk], mbufs[p_ % 2])
            for g0 in range(0, nkt, 8):
                g1 = min(nkt, g0 + 8)
                for kt in range(g0, g1):
                    K.op("pe", lambda e, kt=kt, g0=g0: e.transpose(out=PT[:, kt - g0, :], in_=mb[:, kt * 128:(kt + 1) * 128], identity=identb[:]),
                         reads=[mb.b, identb.b], writes=[PT.b])
                K.op("act", lambda e, g0=g0, g1=g1: e.activation(out=mbT[:, g0:g1, :], in_=PT[:, 0:g1 - g0, :], func=AF.Copy),
                     reads=[PT.b], writes=[mbT.b])
            asteps = [(kt, c) for kt in range(nkt) for c in range(2)]

            def logits(i):
                kt, c = asteps[i]
                pl = PB[5 + (i % 2)]; pts = PTs[i % 2]
                K.op("pe", lambda e: e.matmul(pl[:], lhsT=kT[:, c, kt * 128:(kt + 1) * 128], rhs=qT[:, 4 * c:4 * c + 4, :], start=True, stop=False),
                     reads=[kT.b, qT.b], writes=[pl.b])
                K.op("pe", lambda e: e.matmul(pl[:].rearrange("p (h q) -> p h q", h=4), lhsT=identb[:],
                                              rhs=mbT[:, kt, :].unsqueeze(1).to_broadcast([128, 4, 128]), start=False, stop=True),
                     reads=[identb.b, mbT.b], writes=[pl.b])
                K.op("act", lambda e: e.activation(out=pts[:].rearrange("p h q -> p (h q)"), in_=pl[:], func=AF.Exp, scale=0.125),
                     reads=[pl.b], writes=[pts.b])

            def pv(i):
                kt, c = asteps[i]
                pts = PTs[i % 2]; po = PB[2 + c]
                for hh in range(4):
                    K.op("pe", lambda e, hh=hh: e.matmul(po[:, hh * 65:(hh + 1) * 65], lhsT=pts[:, hh, :], rhs=vaug[:, kt, c, :],
                                                         start=(kt == 0 and hh == 0), stop=(kt == nkt - 1 and hh == 3), skip_group_check=True),
                         reads=[pts.b, vaug.b], writes=[po.b])

            for i in range(len(asteps) + 1):
                if i < len(asteps):
                    logits(i)
                if i >= 1:
                    pv(i - 1)
            for c in range(2):
                po = PB[2 + c]
                pov = po[:, 0:260].rearrange("p (h d) -> p h d", h=4)
                K.op("dve", lambda e, pov=pov, c=c: e.reciprocal(out=rec[:, 4 * c:4 * c + 4], in_=pov[:, :, 64]),
                     reads=[po.b], writes=[rec.b])
                for hh in range(4):
                    K.op("dve", lambda e, pov=pov, c=c, hh=hh: e.tensor_scalar(out=yb[:, 4 * c + hh, :], in0=pov[:, hh, 0:64],
                                                                               scalar1=rec[:, 4 * c + hh:4 * c + hh + 1], scalar2=None,
                                                                               op0=ALU.mult), reads=[po.b, rec.b], writes=[yb.b])
            if "yattn" in dbg:
                ydbg = sb(esB, "ydbg%d" % j, [128, 512])
                K.op("dve", lambda e, ydbg=ydbg: e.tensor_copy(out=ydbg[:], in_=yb[:].rearrange("p h d -> p (h d)")), reads=[yb.b], writes=[ydbg.b])
                K.dma("sp", lambda e, j=j, ydbg=ydbg: e.dma_start(out=dbg_out["yattn"][j], in_=ydbg[:]), reads=[ydbg.b])
            for m in range(4):
                K.op("pe", lambda e, m=m: e.transpose(out=PT[:, m, :], in_=yb[:, 2 * m:2 * m + 2, :].rearrange("p h d -> p (h d)"),
                                                      identity=identb[:]), reads=[yb.b, identb.b], writes=[PT.b])
            K.op("act", lambda e, j=j: e.activation(out=yaT[:, :, j * 128:(j + 1) * 128], in_=PT[:, 0:4, :], func=AF.Copy),
                 reads=[PT.b], writes=[yaT.b])

        for t in range(nq + 2):
            if t < nq:
                stA(t)
            if 1 <= t <= nq:
                stB(t - 1)
            if t >= 2:
                stC(t - 2)
        K.barrier()
        esB.close()
        esAB.close()

        if stop_after == "B":
            K.barrier()
            K.emit()
            K.close()
            top.close()
            return nc

        esC = ExitStack()
        Wu = sb(esC, "Wu", [128, 8, 512], BF16)
        Wga = sb(esC, "Wga", [128, 8, 1024], BF16); Wgp = sb(esC, "Wgp", [128, 8, 1024], BF16)
        Wua = sb(esC, "Wua", [128, 4, 1024], BF16); Wup = sb(esC, "Wup", [128, 4, 1024], BF16)
        Wo = sb(esC, "Wo", [128, 8, 1024], BF16)
        Wpl = sb(esC, "Wpl", [128, 4, 128], BF16)
        psc = sb(esC, "psc", [128, 4]); hm = sb(esC, "hm", [128, 2]); pdv = sb(esC, "pdv", [128, 4, 16])
        K.dma("pool", lambda e: e.dma_start(out=Wu[:], in_=wslice(1352, 1864)), writes=[Wu.b])
        K.dma("pool", lambda e: e.dma_start(out=Wga[:], in_=wslice(1864, 2888)), writes=[Wga.b])
        K.dma("pool", lambda e: e.dma_start(out=Wgp[:], in_=wslice(2888, 3912)), writes=[Wgp.b])
        K.dma("pool", lambda e: e.dma_start(out=Wua[:], in_=w_up_attn.rearrange("(k p) n -> p k n", p=128)), writes=[Wua.b])
        K.dma("pool", lambda e: e.dma_start(out=Wup[:], in_=w_up_pool.rearrange("(k p) n -> p k n", p=128)), writes=[Wup.b])
        K.dma("pool", lambda e: e.dma_start(out=Wo[:], in_=w_out.rearrange("(k p) n -> p k n", p=128)), writes=[Wo.b])
        K.dma("pool", lambda e: e.dma_start(out=Wpl[:], in_=pool_w.rearrange("g c d -> c g d")), writes=[Wpl.b])
        K.dma("sp", lambda e: e.dma_start(out=psc[:], in_=pool_scale.rearrange("(g p) -> p g", p=128), allow_slow_non_contiguous=True), writes=[psc.b])
        K.dma("sp", lambda e: e.dma_start(out=hm[:], in_=hmix), writes=[hm.b])
        K.dma("sp", lambda e: e.dma_start(out=pdv[:].rearrange("p g t -> p (g t)"), in_=pdiv0), writes=[pdv.b])
        load_ln(ln1_g, ln1_b)
        TG = min(4, nq)
        NTK = TG * 128
        xTc = sb(esC, "xTc", [128, 8, NTK], BF16)
        uT = sb(esC, "uT", [128, 4, 144]); T2 = sb(esC, "T2", [128, 4, 144]); T4 = sb(esC, "T4", [128, 4, 144])
        T8 = sb(esC, "T8", [128, 4, 144]); T16 = sb(esC, "T16", [128, 1, 144])
        htmp = sb(esC, "htmp", [128, 4, 16]); dtl = sb(esC, "dtl", [128, 4, NTK], BF16); tmpd = sb(esC, "tmpd", [128, 4, 16])
        ypT = sb(esC, "ypT", [128, 4, NTK], BF16)
        sg = [sb(esC, "sg%d" % i, [128, NTK], BF16) for i in range(2)]
        pr = [sb(esC, "pr%d" % i, [128, NTK], BF16) for i in range(2)]
        mgT = sb(esC, "mgT", [128, 8, NTK], BF16)
        WIN = (2, 4, 8, 16)

        for j0 in range(0, nq, TG):
            for jj in range(TG):
                transpose_x(xres[j0 + jj], V(xTc[:, :, jj * 128:(jj + 1) * 128], xTc.b))
            for g in range(4):
                for k in range(8):
                    K.op("pe", lambda e, g=g, k=k: e.matmul(PB[2 + g][:, 0:NTK], lhsT=Wu[:, k, g * 128:(g + 1) * 128], rhs=xTc[:, k, :],
                                                            start=(k == 0), stop=(k == 7)), reads=[Wu.b, xTc.b], writes=[PB[2 + g].b])
            for jj in range(TG):
                j = j0 + jj
                ts_ = slice(jj * 128, (jj + 1) * 128)
                for g in range(4):
                    K.op("act", lambda e, g=g, ts_=ts_: e.activation(out=uT[:, g, 16:144], in_=PB[2 + g][:, ts_], func=AF.Copy),
                         reads=[PB[2 + g].b], writes=[uT.b])
                if j == 0:
                    K.op("dve", lambda e: e.tensor_scalar(out=uT[:, :, 0:16], in0=uh[:, 0, :, :], scalar1=hm[:, 1:2], scalar2=None, op0=ALU.mult),
                         reads=[uh.b, hm.b], writes=[uT.b])
                else:
                    K.op("dve", lambda e, j=j: e.tensor_scalar(out=htmp[:], in0=uh[:, j - 1, :, :], scalar1=hm[:, 0:1], scalar2=None, op0=ALU.mult),
                         reads=[uh.b, hm.b], writes=[htmp.b])
                    K.op("dve", lambda e, j=j: e.scalar_tensor_tensor(out=uT[:, :, 0:16], in0=uh[:, j, :, :], scalar=hm[:, 1:2], in1=htmp[:],
                                                                      op0=ALU.mult, op1=ALU.add), reads=[uh.b, hm.b, htmp.b], writes=[uT.b])
                K.op("dve", lambda e: e.tensor_tensor(out=T2[:, :, 1:144], in0=uT[:, :, 1:144], in1=uT[:, :, 0:143], op=ALU.add), reads=[uT.b], writes=[T2.b])
                K.op("dve", lambda e: e.tensor_tensor(out=T4[:, 1:4, 3:144], in0=T2[:, 1:4, 3:144], in1=T2[:, 1:4, 1:142], op=ALU.add), reads=[T2.b], writes=[T4.b])
                K.op("dve", lambda e: e.tensor_tensor(out=T8[:, 2:4, 7:144], in0=T4[:, 2:4, 7:144], in1=T4[:, 2:4, 3:140], op=ALU.add), reads=[T4.b], writes=[T8.b])
                K.op("dve", lambda e: e.tensor_tensor(out=T16[:, 0:1, 15:144], in0=T8[:, 3:4, 15:144], in1=T8[:, 3:4, 7:136], op=ALU.add), reads=[T8.b], writes=[T16.b])
                Sg = (T2, T4, T8, T16)
                Si = (0, 1, 2, 0)
                for g in range(4):
                    K.op("dve", lambda e, g=g, ts_=ts_: e.scalar_tensor_tensor(out=dtl[:, g, ts_], in0=Sg[g][:, Si[g], 16:144], scalar=1.0 / WIN[g], in1=uT[:, g, 16:144],
                                                                              op0=ALU.mult, op1=ALU.subtract), reads=[Sg[g].b, uT.b], writes=[dtl.b])
                if j == 0:
                    for g in range(4):
                        K.op("dve", lambda e, g=g: e.tensor_tensor(out=tmpd[:, g, :], in0=Sg[g][:, Si[g], 16:32], in1=pdv[:, g, :], op=ALU.mult),
                             reads=[Sg[g].b, pdv.b], writes=[tmpd.b])
                    K.op("dve", lambda e: e.tensor_tensor(out=dtl[:, :, 0:16], in0=tmpd[:], in1=uT[:, :, 16:32], op=ALU.subtract),
                         reads=[tmpd.b, uT.b], writes=[dtl.b])
            for g in range(4):
                pbp = PB[2 + (g % 2)]
                K.op("pe", lambda e, g=g, pbp=pbp: e.matmul(pbp[:, 0:NTK], lhsT=Wpl[:, g, :], rhs=dtl[:, g, :], start=True, stop=True),
                     reads=[Wpl.b, dtl.b], writes=[pbp.b])
                K.op("act", lambda e, g=g, pbp=pbp: e.activation(out=ypT[:, g, :], in_=pbp[:, 0:NTK], func=AF.Identity, scale=psc[:, g:g + 1]),
                     reads=[pbp.b, psc.b], writes=[ypT.b])
            bi = 0
            for m in range(8):
                ms = slice(m * 128, (m + 1) * 128)
                for br in range(2):
                    pu = PB[(bi % 3) * 2]; pg_ = PB[(bi % 3) * 2 + 1]
                    sgm = sg[bi % 2]; prm = pr[bi % 2]; bi += 1
                    Wup_ = Wua if br == 0 else Wup
                    Wg_ = Wga if br == 0 else Wgp
                    for kb in range(4):
                        rhs_t, rhs_b = ((yaT[:, kb, j0 * 128:j0 * 128 + NTK], yaT.b) if br == 0 else (ypT[:, kb, :], ypT.b))
                        K.op("pe", lambda e, pu=pu, kb=kb, ms=ms, Wup_=Wup_, rhs_t=rhs_t: e.matmul(pu[:, 0:NTK], lhsT=Wup_[:, kb, ms], rhs=rhs_t,
                                                                                                  start=(kb == 0), stop=(kb == 3)),
                             reads=[Wup_.b, rhs_b], writes=[pu.b])
                    for k in range(8):
                        K.op("pe", lambda e, pg_=pg_, k=k, ms=ms, Wg_=Wg_: e.matmul(pg_[:, 0:NTK], lhsT=Wg_[:, k, ms], rhs=xTc[:, k, :],
                                                                                   start=(k == 0), stop=(k == 7)), reads=[Wg_.b, xTc.b], writes=[pg_.b])
                    K.op("act", lambda e, pg_=pg_, sgm=sgm: e.activation(out=sgm[:], in_=pg_[:, 0:NTK], func=AF.Sigmoid), reads=[pg_.b], writes=[sgm.b])
                    if br == 0:
                        K.op("dve", lambda e, pu=pu, sgm=sgm, prm=prm: e.tensor_tensor(out=prm[:], in0=pu[:, 0:NTK], in1=sgm[:], op=ALU.mult),
                             reads=[pu.b, sgm.b], writes=[prm.b])
                        pr_a = prm
                    else:
                        K.op("dve", lambda e, pu=pu, sgm=sgm, prm=prm: e.tensor_tensor(out=prm[:], in0=pu[:, 0:NTK], in1=sgm[:], op=ALU.mult),
                             reads=[pu.b, sgm.b], writes=[prm.b])
                        K.op("dve", lambda e, prm=prm, pr_a=pr_a, m=m: e.tensor_tensor(out=mgT[:, m, :], in0=pr_a[:], in1=prm[:], op=ALU.add),
                             reads=[pr_a.b, prm.b], writes=[mgT.b])
            for jj in range(TG):
                j = j0 + jj
                for n in range(2):
                    pbo = PB[(jj * 2 + n) % 6]
                    for m in range(8):
                        K.op("pe", lambda e, pbo=pbo, m=m, n=n, jj=jj: e.matmul(pbo[:], lhsT=mgT[:, m, jj * 128:(jj + 1) * 128], rhs=Wo[:, m, n * 512:(n + 1) * 512],
                                                                               start=(m == 0), stop=(m == 7)), reads=[mgT.b, Wo.b], writes=[pbo.b])
                    K.op("dve", lambda e, pbo=pbo, n=n, j=j: e.scalar_tensor_tensor(out=xres[j][:, n * 512:(n + 1) * 512], in0=xres[j][:, n * 512:(n + 1) * 512],
                                                                                   scalar=ALPHA, in1=pbo[:], op0=ALU.mult, op1=ALU.add),
                         reads=[xres[j].b, pbo.b], writes=[xres[j].b])
                layer_norm(xres[j])
        K.barrier()
        esC.close()
        esY.close()
        if stop_after == "C":
            K.emit(); K.close(); top.close()
            return nc

        esD1 = ExitStack()
        Wpg = sb(esD1, "Wpg", [128, 8, 1024], BF16); Wpp = sb(esD1, "Wpp", [128, 2, 1024], BF16)
        Wr = sb(esD1, "Wr", [128, 8, 32]); rbr = sb(esD1, "rbr", [1, 32]); ones_f = sb(esD1, "ones_f", [1, 128])
        Ust = sb(esD1, "Ust", [128, 128], BF16); ecap = sb(esD1, "ecap", [128, 32]); base = sb(esD1, "base", [128, 32])
        zer = sb(esD1, "zer", [128, 32])
        K.dma("pool", lambda e: e.dma_start(out=Wpg[:], in_=ple_w_gate.rearrange("(k p) n -> p k n", p=128)), writes=[Wpg.b])
        K.dma("pool", lambda e: e.dma_start(out=Wpp[:], in_=ple_w_proj.rearrange("(k p) n -> p k n", p=128)), writes=[Wpp.b])
        K.dma("sp", lambda e: e.dma_start(out=Wr[:], in_=router_w.rearrange("(k p) n -> p k n", p=128)), writes=[Wr.b])
        K.dma("sp", lambda e: e.dma_start(out=rbr[:], in_=router_b.rearrange("(a n) -> a n", a=1)), writes=[rbr.b])
        K.op("dve", lambda e: e.memset(ones_f[:], 1.0), writes=[ones_f.b])
        K.op("dve", lambda e: e.memset(base[:], 0.0), writes=[base.b])
        K.op("dve", lambda e: e.memset(zer[:], 0.0), writes=[zer.b])
        K.op("dve", lambda e: e.tensor_scalar(out=Ust[:], in0=iot[:], scalar1=pidx[:, 0:1], scalar2=None, op0=ALU.is_gt),
             reads=[iot.b, pidx.b], writes=[Ust.b])
        K.op("pool", lambda e: e.iota(ecap[:], pattern=[[CAP, 32]], base=0, channel_multiplier=0, allow_small_or_imprecise_dtypes=True),
             writes=[ecap.b])
        x1T = sb(esD1, "x1T", [128, 8, 128], BF16); x1Tf = sb(esD1, "x1Tf", [128, 8, 128])
        x1b = [sb(esD1, "x1b%d" % i, [128, D], BF16) for i in range(2)]
        lg = sb(esD1, "lg", [128, 32]); top8 = sb(esD1, "top8", [128, 8]); msk = sb(esD1, "msk", [128, 32])
        mskb = sb(esD1, "mskb", [128, 32], BF16); nv1 = sb(esD1, "nv1", [128, 1]); ex = sb(esD1, "ex", [128, 32])
        den = sb(esD1, "den", [128, 1]); G = sb(esD1, "G", [128, 32]); slotf = sb(esD1, "slotf", [128, 32])
        incl = sb(esD1, "incl", [128, 32]); rank = sb(esD1, "rank", [128, 32]); selk = sb(esD1, "selk", [128, 32])
        tm32 = sb(esD1, "tm32", [128, 32]); sk4 = sb(esD1, "sk4", [128, 4])
        ptl = sb(esD1, "ptl", [128, 256]); pTb = sb(esD1, "pTb", [128, 2, 128], BF16); sgp = sb(esD1, "sgp", [128, D])
        for j in range(nq):
            xb_ = x1b[j % 2]
            transpose_x(xres[j], x1T, x1Tf)
            K.op("act", lambda e, xb_=xb_, j=j: e.activation(out=xb_[:], in_=xres[j][:], func=AF.Copy), reads=[xres[j].b], writes=[xb_.b])
            for k in range(8):
                K.op("pe", lambda e, k=k: e.matmul(PB[2][:, 0:32], lhsT=x1Tf[:, k, :], rhs=Wr[:, k, :], start=(k == 0), stop=False),
                     reads=[x1Tf.b, Wr.b], writes=[PB[2].b])
            K.op("pe", lambda e: e.matmul(PB[2][:, 0:32], lhsT=ones_f[0:1, :], rhs=rbr[0:1, :], start=False, stop=True),
                 reads=[ones_f.b, rbr.b], writes=[PB[2].b])
            K.op("dve", lambda e: e.tensor_copy(out=lg[:], in_=PB[2][:, 0:32]), reads=[PB[2].b], writes=[lg.b])
            K.op("dve", lambda e: e.max(out=top8[:], in_=lg[:]), reads=[lg.b], writes=[top8.b])
            K.op("dve", lambda e: e.tensor_scalar(out=msk[:], in0=lg[:], scalar1=top8[:, 3:4], scalar2=None, op0=ALU.is_ge), reads=[lg.b, top8.b], writes=[msk.b])
            K.op("dve", lambda e: e.tensor_copy(out=mskb[:], in_=msk[:]), reads=[msk.b], writes=[mskb.b])
            K.op("dve", lambda e: e.tensor_scalar(out=nv1[:], in0=top8[:, 0:1], scalar1=-1.0, scalar2=None, op0=ALU.mult), reads=[top8.b], writes=[nv1.b])
            K.op("act", lambda e: e.activation(out=ex[:], in_=lg[:], func=AF.Exp, bias=nv1[:, 0:1]), reads=[lg.b, nv1.b], writes=[ex.b])
            K.op("dve", lambda e: e.tensor_tensor(out=ex[:], in0=ex[:], in1=msk[:], op=ALU.mult), reads=[ex.b, msk.b], writes=[ex.b])
            K.op("dve", lambda e: e.tensor_reduce(out=den[:], in_=ex[:], axis=AX.X, op=ALU.add), reads=[ex.b], writes=[den.b])
            K.op("dve", lambda e: e.reciprocal(out=den[:], in_=den[:]), reads=[den.b], writes=[den.b])
            K.op("dve", lambda e: e.tensor_scalar(out=G[:], in0=ex[:], scalar1=den[:, 0:1], scalar2=None, op0=ALU.mult), reads=[ex.b, den.b], writes=[G.b])
            K.op("pe", lambda e: e.matmul(PB[3][:, 0:32], lhsT=Ust[:], rhs=mskb[:], start=True, stop=True), reads=[Ust.b, mskb.b], writes=[PB[3].b])
            K.op("pe", lambda e: e.matmul(PB[3][:, 32:64], lhsT=ones_b[:], rhs=mskb[:], start=True, stop=True), reads=[ones_b.b, mskb.b], writes=[PB[3].b])
            K.op("dve", lambda e: e.tensor_tensor(out=slotf[:], in0=PB[3][:, 0:32], in1=base[:], op=ALU.add), reads=[PB[3].b, base.b], writes=[slotf.b])
            K.op("dve", lambda e: e.tensor_tensor(out=slotf[:], in0=slotf[:], in1=ecap[:], op=ALU.add), reads=[slotf.b, ecap.b], writes=[slotf.b])
            K.op("dve", lambda e: e.tensor_tensor(out=base[:], in0=PB[3][:, 32:64], in1=base[:], op=ALU.add), reads=[PB[3].b, base.b], writes=[base.b])
            K.op("dve", lambda e: e.tensor_tensor_scan(out=incl[:], data0=msk[:], data1=zer[:], initial=0.0, op0=ALU.add, op1=ALU.add),
                 reads=[msk.b, zer.b], writes=[incl.b])
            K.op("dve", lambda e: e.tensor_tensor(out=rank[:], in0=incl[:], in1=msk[:], op=ALU.subtract), reads=[incl.b, msk.b], writes=[rank.b])
            for k4 in range(4):
                K.op("dve", lambda e, k4=k4: e.scalar_tensor_tensor(out=selk[:], in0=rank[:], scalar=float(k4), in1=msk[:], op0=ALU.is_equal, op1=ALU.mult),
                     reads=[rank.b, msk.b], writes=[selk.b])
                K.op("dve", lambda e: e.tensor_tensor(out=tm32[:], in0=selk[:], in1=slotf[:], op=ALU.mult), reads=[selk.b, slotf.b], writes=[tm32.b])
                K.op("dve", lambda e, k4=k4: e.tensor_reduce(out=sk4[:, k4:k4 + 1], in_=tm32[:], axis=AX.X, op=ALU.add), reads=[tm32.b], writes=[sk4.b])
                K.op("dve", lambda e: e.tensor_tensor(out=tm32[:], in0=selk[:], in1=G[:], op=ALU.mult), reads=[selk.b, G.b], writes=[tm32.b])
                K.op("dve", lambda e, k4=k4, j=j: e.tensor_reduce(out=gates[:, j, k4:k4 + 1], in_=tm32[:], axis=AX.X, op=ALU.add), reads=[tm32.b], writes=[gates.b])
            K.op("dve", lambda e, j=j: e.tensor_copy(out=slots[:, j, :], in_=sk4[:]), reads=[sk4.b], writes=[slots.b])
            for k4 in range(4):
                K.dma("pool", lambda e, k4=k4, j=j, xb_=xb_: e.indirect_dma_start(out=Xs, out_offset=bass.IndirectOffsetOnAxis(slots[:, j, k4:k4 + 1], 0),
                                                                                 in_=xb_[:], in_offset=None), reads=[xb_.b, slots.b], writes=[bXs])
            K.dma("sp", lambda e, j=j: e.dma_start(out=ptl[:], in_=pown[j]), writes=[ptl.b])
            for k in range(2):
                K.op("pe", lambda e, k=k: e.transpose(out=PB[4][:, k * 128:(k + 1) * 128], in_=ptl[:, k * 128:(k + 1) * 128], identity=identf[:]),
                     reads=[ptl.b, identf.b], writes=[PB[4].b])
            K.op("act", lambda e: e.activation(out=pTb[:], in_=PB[4][:, 0:256].rearrange("p (k t) -> p k t", k=2), func=AF.Copy), reads=[PB[4].b], writes=[pTb.b])
            for n in range(2):
                for k in range(8):
                    K.op("pe", lambda e, n=n, k=k: e.matmul(PB[5 + n][:], lhsT=x1T[:, k, :], rhs=Wpg[:, k, n * 512:(n + 1) * 512], start=(k == 0), stop=(k == 7)),
                         reads=[x1T.b, Wpg.b], writes=[PB[5 + n].b])
                K.op("act", lambda e, n=n: e.activation(out=sgp[:, n * 512:(n + 1) * 512], in_=PB[5 + n][:], func=AF.Sigmoid), reads=[PB[5 + n].b], writes=[sgp.b])
                for k in range(2):
                    K.op("pe", lambda e, n=n, k=k: e.matmul(PB[n][:], lhsT=pTb[:, k, :], rhs=Wpp[:, k, n * 512:(n + 1) * 512], start=(k == 0), stop=(k == 1)),
                         reads=[pTb.b, Wpp.b], writes=[PB[n].b])
                K.op("dve", lambda e, n=n: e.tensor_tensor(out=sgp[:, n * 512:(n + 1) * 512], in0=PB[n][:], in1=sgp[:, n * 512:(n + 1) * 512], op=ALU.mult),
                     reads=[PB[n].b, sgp.b], writes=[sgp.b])
            K.op("dve", lambda e, j=j: e.scalar_tensor_tensor(out=xres[j][:], in0=xres[j][:], scalar=ALPHA, in1=sgp[:], op0=ALU.mult, op1=ALU.add),
                 reads=[xres[j].b, sgp.b], writes=[xres[j].b])
        K.barrier()
        esD1.close()

        esD2 = ExitStack()
        NR = 16
        ring = [sb(esD2, "ring%d" % i, [128, 8, 256], BF16) for i in range(NR)]
        bgur = [sb(esD2, "bgur%d" % i, [16, 128]) for i in range(2)]
        bg = [sb(esD2, "bg%d" % i, [128, 16]) for i in range(2)]
        bu1 = [sb(esD2, "bu1%d" % i, [128, 8]) for i in range(2)]
        bdb = [sb(esD2, "bdb%d" % i, [1, D], BF16) for i in range(2)]
        xgs = [sb(esD2, "xg%d" % i, [128, 3, D], BF16) for i in range(2)]
        XsTs = [sb(esD2, "XsT%d" % i, [128, 8, CAP], BF16) for i in range(2)]
        actT = sb(esD2, "actT", [128, 8, CAP], BF16)
        gt = [sb(esD2, "gt%d" % i, [128, CAP]) for i in range(2)]; sgx = [sb(esD2, "sgx%d" % i, [128, CAP]) for i in range(2)]
        uA = [sb(esD2, "uA%d" % i, [128, CAP]) for i in range(2)]; gs = [sb(esD2, "gs%d" % i, [128, CAP]) for i in range(2)]
        ysb = [sb(esD2, "ysb%d" % i, [128, D]) for i in range(2)]
        nexp = NEXP
        ring_i = [0]
        yi = [0]
        UNITS = {}

        ORDER = [("g", 0), ("u", 0), ("g", 1), ("u", 1), ("g", 2), ("u", 2), ("g", 3), ("u", 3), ("d", 0), ("d", 1), ("d", 2), ("d", 3)]
        for ex_ in range(nexp):
            UNITS[ex_] = {kq: ring[(12 * ex_ + pos) % NR] for pos, kq in enumerate(ORDER)}
        nxt = [0]

        def issue_loads(n):
            for _ in range(n):
                g = nxt[0]
                if g >= 12 * nexp:
                    return
                nxt[0] += 1
                ex_, pos = divmod(g, 12)
                kind, q = ORDER[pos]
                r = ring[g % NR]
                if kind == "g":
                    src = exp_w_gu[ex_][:, q * 256:(q + 1) * 256]
                elif kind == "u":
                    src = exp_w_gu[ex_][:, 1024 + q * 256:1024 + (q + 1) * 256]
                else:
                    src = exp_w_down[ex_][:, q * 256:(q + 1) * 256]
                K.dma("pool", lambda e, r=r, src=src: e.dma_start(out=r[:], in_=src.rearrange("(k p) n -> p k n", p=128)), writes=[r.b])

        def load_bias(ex_):
            bdx = bdb[ex_ % 2]
            K.dma("pool", lambda e: e.dma_start(out=bdx[:], in_=exp_b_down[ex_].rearrange("(a n) -> a n", a=1)), writes=[bdx.b])

        def prep(ex_):
            xg = xgs[ex_ % 2]; XsT = XsTs[ex_ % 2]
            bgr = bgur[ex_ % 2]; bgx = bg[ex_ % 2]; bux = bu1[ex_ % 2]
            K.dma("sp", lambda e: e.dma_start(out=bgr[:], in_=exp_b_gu[ex_].rearrange("(i p) -> i p", p=128)), writes=[bgr.b])
            K.dma("sp", lambda e: e.dma_start(out=xg[:], in_=Xs[ex_ * CAP:(ex_ + 1) * CAP, :].rearrange("(b p) n -> p b n", p=128)),
                  reads=[bXs], writes=[xg.b])
            K.op("pe", lambda e: e.transpose(out=PB[6][:, 0:16], in_=bgr[0:16, :], identity=identf[0:16, 0:16]),
                 reads=[bgr.b, identf.b], writes=[PB[6].b])
            K.op("act", lambda e: e.activation(out=bgx[:], in_=PB[6][:, 0:16], func=AF.Copy), reads=[PB[6].b], writes=[bgx.b])
            K.op("dve", lambda e: e.tensor_scalar(out=bux[:], in0=bgx[:, 8:16], scalar1=1.0, scalar2=None, op0=ALU.add),
                 reads=[bgx.b], writes=[bux.b])
            for blk in range(3):
                for kc in range(8):
                    K.op("pe", lambda e, blk=blk, kc=kc: e.transpose(out=PT[:, kc, :], in_=xg[:, blk, kc * 128:(kc + 1) * 128], identity=identb[:]),
                         reads=[xg.b, identb.b], writes=[PT.b])
                K.op("act", lambda e, blk=blk: e.activation(out=XsT[:, :, blk * 128:(blk + 1) * 128], in_=PT[:], func=AF.Copy), reads=[PT.b], writes=[XsT.b])

        def gate_up(ex_):
            us = UNITS[ex_]; XsT = XsTs[ex_ % 2]; bgx = bg[ex_ % 2]; bux = bu1[ex_ % 2]
            for i in range(8):
                pgt = PB[(i % 2) * 2]; put = PB[(i % 2) * 2 + 1]
                gti = gt[i % 2]; sgi = sgx[i % 2]; uAi = uA[i % 2]; gsi = gs[i % 2]
                cs_ = slice((i % 2) * 128, (i % 2 + 1) * 128)
                rg = us[("g", i // 2)]; ru = us[("u", i // 2)]
                for kc in range(8):
                    K.op("pe", lambda e, kc=kc, pgt=pgt, rg=rg, cs_=cs_: e.matmul(pgt[:, 0:CAP], lhsT=rg[:, kc, cs_], rhs=XsT[:, kc, :], start=(kc == 0), stop=(kc == 7)),
                         reads=[rg.b, XsT.b], writes=[pgt.b])
                for kc in range(8):
                    K.op("pe", lambda e, kc=kc, put=put, ru=ru, cs_=cs_: e.matmul(put[:, 0:CAP], lhsT=ru[:, kc, cs_], rhs=XsT[:, kc, :], start=(kc == 0), stop=(kc == 7)),
                         reads=[ru.b, XsT.b], writes=[put.b])
                K.op("dve", lambda e, i=i, gti=gti, pgt=pgt: e.tensor_scalar(out=gti[:], in0=pgt[:, 0:CAP], scalar1=bgx[:, i:i + 1], scalar2=7.0, op0=ALU.add, op1=ALU.min),
                     reads=[pgt.b, bgx.b], writes=[gti.b])
                K.op("act", lambda e, sgi=sgi, gti=gti: e.activation(out=sgi[:], in_=gti[:], func=AF.Sigmoid, scale=1.702), reads=[gti.b], writes=[sgi.b])
                K.op("dve", lambda e, i=i, uAi=uAi, put=put: e.tensor_scalar(out=uAi[:], in0=put[:, 0:CAP], scalar1=bux[:, i:i + 1], scalar2=8.0, op0=ALU.add, op1=ALU.min),
                     reads=[put.b, bux.b], writes=[uAi.b])
                K.op("dve", lambda e, gsi=gsi, gti=gti, sgi=sgi: e.tensor_tensor(out=gsi[:], in0=gti[:], in1=sgi[:], op=ALU.mult), reads=[gti.b, sgi.b], writes=[gsi.b])
                K.op("dve", lambda e, i=i, uAi=uAi, gsi=gsi: e.scalar_tensor_tensor(out=actT[:, i, :], in0=uAi[:], scalar=-6.0, in1=gsi[:], op0=ALU.max, op1=ALU.mult),
                     reads=[uAi.b, gsi.b], writes=[actT.b])
                if i % 2 == 1:
                    issue_loads(2)

        def down(ex_):
            us = UNITS[ex_]; bdx = bdb[ex_ % 2]
            di = 0
            for rb_ in range(3):
                ys = ysb[yi[0] % 2]; yi[0] += 1
                for half in range(2):
                    pbd = PB[4 + (di % 2)]; di += 1
                    for qq in range(2):
                        r = us[("d", half * 2 + qq)]
                        c0_ = half * 512 + qq * 256
                        K.op("pe", lambda e, qq=qq, pbd=pbd, c0_=c0_: e.matmul(pbd[:, qq * 256:(qq + 1) * 256], lhsT=ones_b[0:1, :], rhs=bdx[0:1, c0_:c0_ + 256],
                                                                             start=True, stop=False), reads=[ones_b.b, bdx.b], writes=[pbd.b])
                        for i in range(8):
                            K.op("pe", lambda e, qq=qq, i=i, r=r, pbd=pbd, rb_=rb_: e.matmul(pbd[:, qq * 256:(qq + 1) * 256], lhsT=actT[:, i, rb_ * 128:(rb_ + 1) * 128], rhs=r[:, i, :],
                                                                                          start=False, stop=(i == 7)),
                                 reads=[actT.b, r.b], writes=[pbd.b])
                    K.op("act", lambda e, half=half, ys=ys, pbd=pbd: e.activation(out=ys[:, half * 512:(half + 1) * 512], in_=pbd[:], func=AF.Copy),
                         reads=[pbd.b], writes=[ys.b])
                K.dma("sp", lambda e, ys=ys, rb_=rb_: e.dma_start(out=Ys[ex_ * CAP + rb_ * 128:ex_ * CAP + (rb_ + 1) * 128, :], in_=ys[:]),
                      reads=[ys.b], writes=[bYs])

        issue_loads(NR)
        load_bias(0)
        prep(0)
        for ex_ in range(nexp):
            gate_up(ex_)
            if ex_ + 1 < nexp:
                load_bias(ex_ + 1)
                prep(ex_ + 1)
            down(ex_)
            issue_loads(4)
        K.barrier()
        esD2.close()

        esD3 = ExitStack()
        yk = [sb(esD3, "yk%d" % i, [128, D]) for i in range(4)]
        if partner:
            xo16 = [sb(esD3, "xo16%d" % i, [128, D], BF16) for i in range(2)]
        load_ln(ln2_g, ln2_b)
        gi = 0
        for j in range(nq):
            for k4 in range(4):
                y = yk[gi % 4]; gi += 1
                K.dma("pool", lambda e, y=y, j=j, k4=k4: e.indirect_dma_start(out=y[:], out_offset=None, in_=Ys,
                                                                             in_offset=bass.IndirectOffsetOnAxis(slots[:, j, k4:k4 + 1], 0)),
                      reads=[bYs, slots.b], writes=[y.b])
                K.op("dve", lambda e, y=y, j=j, k4=k4: e.scalar_tensor_tensor(out=xres[j][:], in0=y[:], scalar=gates[:, j, k4:k4 + 1], in1=xres[j][:],
                                                                             op0=ALU.mult, op1=ALU.add), reads=[y.b, gates.b, xres[j].b], writes=[xres[j].b])
            layer_norm(xres[j])
            if last:
                K.dma("sp", lambda e, j=j: e.dma_start(out=yout[j], in_=xres[j][:]), reads=[xres[j].b])
            elif partner:
                xo = xo16[j % 2]
                K.op("act", lambda e, xo=xo, j=j: e.activation(out=xo[:], in_=xres[j][:], func=AF.Copy), reads=[xres[j].b], writes=[xo.b])
                K.dma("sp", lambda e, xo=xo, j=j: e.dma_start(out=xo1[j * 128:(j + 1) * 128, :], in_=xo[:]), reads=[xo.b], writes=[bxo1])
        K.barrier()
        esD3.close()


    run_layer(0, partner=True, zero_slabs=True)
    run_layer(0)
    run_layer(1)
    K.barrier()
    K.emit()
    K.close()
    top.close()
    return nc


def rope_tables():
    pos = np.arange(4096, dtype=np.float32)
    inv = (np.float32(500000.0) ** (-np.arange(0, 16, 2, dtype=np.float32) / np.float32(16))).astype(np.float32)
    ang = (pos[:, None] * inv[None, :]).astype(np.float32)
    return np.cos(ang).astype(np.float32), np.sin(ang).astype(np.float32)


def core_inputs(x_b, p_b, c, weights):
    cos, sin = rope_tables()
    xt = x_b.reshape(32, 128, D)
    own = np.ascontiguousarray(xt[c::2]); oth = np.ascontiguousarray(xt[(1 - c)::2])
    cs = np.concatenate([np.broadcast_to(cos[:, None, :], (4096, 8, 8)).reshape(4096, 64),
                         np.broadcast_to(sin[:, None, :], (4096, 8, 8)).reshape(4096, 64)], axis=1).reshape(32, 128, 128)
    tri = np.where(np.arange(128)[None, :] <= np.arange(128)[:, None], 0.0, NEG).astype(np.float32)
    othm = np.full((128, 128), 0.0 if c == 1 else NEG, np.float32)
    hm = np.zeros((128, 2), np.float32); hm[:, c] = 1.0
    pd = np.zeros((128, 4, 16), np.float32)
    for g, win in enumerate((2, 4, 8, 16)):
        if c == 0:
            pd[:, g, :] = 1.0 / np.minimum(np.arange(1, 17, dtype=np.float32), float(win))
        else:
            pd[:, g, :] = 1.0 / win
    m = dict(weights)
    c_ = 1 - c
    othm_p = np.full((128, 128), 0.0 if c_ == 1 else NEG, np.float32)
    hm_p = np.zeros((128, 2), np.float32); hm_p[:, c_] = 1.0
    pd_p = np.zeros((128, 4, 16), np.float32)
    for g, win in enumerate((2, 4, 8, 16)):
        pd_p[:, g, :] = (1.0 / np.minimum(np.arange(1, 17, dtype=np.float32), float(win))) if c_ == 0 else (1.0 / win)
    m.update(poth0=np.ascontiguousarray(p_b.reshape(2, 32, 128, 256)[0, c_::2]),
             dmask_p=np.concatenate([tri, othm_p], axis=1), hmix_p=hm_p, pdiv0_p=pd_p.reshape(128, 64))
    m.update(xown=own, xoth=oth, pown=np.ascontiguousarray(p_b.reshape(2, 32, 128, 256)[:, c::2]),
             cs_own=np.ascontiguousarray(cs[c::2]), cs_oth=np.ascontiguousarray(cs[(1 - c)::2]),
             dmask=np.concatenate([tri, othm], axis=1), hmix=hm, pdiv0=pd.reshape(128, 64))
    return m


LAYER_WEIGHTS = ("w_in", "pool_w", "pool_scale", "w_up_attn", "w_up_pool", "w_out", "ln1_g", "ln1_b", "router_w", "router_b",
                 "exp_w_gu", "exp_b_gu", "exp_w_down", "exp_b_down", "ple_w_gate", "ple_w_proj", "ln2_g", "ln2_b")


def kernel(**inputs):
    x = np.asarray(inputs["x"], dtype=np.float32)
    p = np.asarray(inputs["p"], dtype=np.float32)
    nc = build()
    w = {}
    for k in LAYER_WEIGHTS:
        a = np.asarray(inputs[k], dtype=np.float32)
        if k in ("exp_w_gu", "exp_w_down"):
            for i in range(2):
                w["%s_%d" % (k, i)] = np.ascontiguousarray(a[i])
        else:
            w[k] = np.ascontiguousarray(a)
    w["ln0_g"] = np.asarray(inputs["ln0_g"], dtype=np.float32)
    w["ln0_b"] = np.asarray(inputs["ln0_b"], dtype=np.float32)
    maps = []
    for core in range(8):
        b, c = core // 2, core % 2
        maps.append(core_inputs(x[b], p[:, b], c, w))
    res = run_bass_kernel_spmd(nc, maps, core_ids=list(range(8)))
    out = np.empty_like(x)
    for core in range(8):
        b, c = core // 2, core % 2
        out[b].reshape(32, 128, D)[c::2] = np.asarray(res.results[core]["yout"]).reshape(NT, 128, D)
    return out
```

```python
from contextlib import ExitStack
import os
SKIP = set(os.environ.get('KSKIP', '').split(','))
import numpy as np
import concourse.bass as bass
import concourse.mybir as mybir
from concourse.bass_utils import run_bass_kernel_spmd

F32 = mybir.dt.float32
BF16 = mybir.dt.bfloat16
I32 = mybir.dt.int32
ALU = mybir.AluOpType
AF = mybir.ActivationFunctionType
AX = mybir.AxisListType

SAME_SYNC = True
NDMASEM = 12

D = 1024
NT = 16
CAP = 384
NEXP = 32
NIT = 16
ALPHA = 4.0 ** 0.25
EPS = 1e-5
IDX_SCALE = (64 ** -0.5) * (8 ** -0.5)
NEG = -1.0e30
MASKV = -240000.0


class Buf:
    __slots__ = ("name", "w", "r", "excl")

    def __init__(self, name="", excl=False):
        self.name = name
        self.w = None
        self.r = []
        self.excl = excl


class Stream:
    def __init__(self, name, eng):
        self.name = name
        self.eng = eng
        self.items = []
        self.waited = {}
        self.count = 0
        self.dma_i = 0


class Kern:
    def __init__(self, nc):
        self.nc = nc
        self.sems = {}
        self.sem_ctx = []
        self.streams = {}
        for name, eng in (("pe", nc.tensor), ("act", nc.scalar), ("dve", nc.vector),
                          ("pool", nc.gpsimd), ("sp", nc.sync)):
            self.streams[name] = Stream(name, eng)
        self.dma_cnt = {}
        self.nops = 0

    def sem(self, key):
        if key not in self.sems:
            cm = self.nc.semaphore("s_" + key)
            h = cm.__enter__()
            self.sem_ctx.append(cm)
            self.sems[key] = h
        return self.sems[key]

    def _wait(self, st, ev):
        if ev is None:
            return
        key, val = ev
        if st.waited.get(key, 0) >= val:
            return
        st.waited[key] = val
        sem = self.sem(key)
        st.items.append(lambda e, sem=sem, val=val: e.wait_ge(sem, val))

    @staticmethod
    def _deps(reads, writes):
        deps = []
        for b in reads:
            if b.w is not None:
                deps.append(b.w)
            if b.excl:
                deps.extend(b.r)
        for b in writes:
            if b.w is not None:
                deps.append(b.w)
            deps.extend(b.r)
        return deps

    @staticmethod
    def _mark(ev, reads, writes):
        for b in reads:
            if b.excl:
                b.w = ev
                b.r = []
            else:
                b.r.append(ev)
        for b in writes:
            b.w = ev
            b.r = []

    def op(self, eng, fn, reads=(), writes=()):
        st = self.streams[eng]
        own = "e_" + eng
        for ev in self._deps(reads, writes):
            if ev[0] == own and (eng == "pe" or not SAME_SYNC):
                continue
            self._wait(st, ev)
        st.count += 1
        ev = (own, st.count)
        sem = self.sem(own)
        st.items.append(lambda e, fn=fn, sem=sem: fn(e).then_inc(sem, 1))
        self._mark(ev, reads, writes)
        self.nops += 1
        return ev

    def dma(self, q, fn, reads=(), writes=()):
        st = self.streams[q]
        slot = st.dma_i % NDMASEM
        st.dma_i += 1
        key = "d_%s_%d" % (q, slot)
        prev = self.dma_cnt.get(key, 0)
        if prev:
            self._wait(st, (key, prev))
        for ev in self._deps(reads, writes):
            self._wait(st, ev)
        val = prev + 16
        self.dma_cnt[key] = val
        ev = (key, val)
        sem = self.sem(key)
        st.items.append(lambda e, fn=fn, sem=sem: fn(e).then_inc(sem, 16))
        self._mark(ev, reads, writes)
        self.nops += 1
        return ev

    def coll(self, fn, reads=(), writes=()):
        st = self.streams["pool"]
        key = "coll"
        prev = self.dma_cnt.get(key, 0)
        if prev:
            self._wait(st, (key, prev))
        for ev in self._deps(reads, writes):
            self._wait(st, ev)
        val = prev + 16
        self.dma_cnt[key] = val
        ev = (key, val)
        sem = self.sem(key)
        st.items.append(lambda e, fn=fn, sem=sem: fn(e).then_inc(sem, 16))
        self._mark(ev, reads, writes)
        return ev

    def barrier(self):
        for st in self.streams.values():
            for o in self.streams.values():
                if o.count:
                    self._wait(st, ("e_" + o.name, o.count))
            for key, val in self.dma_cnt.items():
                self._wait(st, (key, val))

    def emit(self):
        nc = self.nc
        with nc.Block() as block:
            def mk(st):
                def body(e):
                    for it in st.items:
                        it(e)
                return body
            block.tensor(mk(self.streams["pe"]))
            block.scalar(mk(self.streams["act"]))
            block.vector(mk(self.streams["dve"]))
            block.gpsimd(mk(self.streams["pool"]))
            block.sync(mk(self.streams["sp"]))

    def close(self):
        for cm in reversed(self.sem_ctx):
            cm.__exit__(None, None, None)


class T:
    def __init__(self, t, name, excl=False):
        self.t = t
        self.b = Buf(name, excl)

    def __getitem__(self, k):
        return self.t[k]


def build(layers=(0, 1), nq=NT, dbg=()):
    stop_after = "D"
    nc = bass.Bass("TRN2", target_bir_lowering=False)
    K = Kern(nc)

    def din(name, shape, dt=F32):
        return nc.dram_tensor(name, list(shape), dt, kind="ExternalInput").ap()

    def dout(name, shape, dt=F32):
        return nc.dram_tensor(name, list(shape), dt, kind="ExternalOutput").ap()

    xown = din("xown", [NT, 128, D])
    xoth = din("xoth", [NT, 128, D])
    pown_all = din("pown", [2, NT, 128, 256])
    cs_own = din("cs_own", [NT, 128, 128])
    cs_oth = din("cs_oth", [NT, 128, 128])
    dmask = din("dmask", [128, 256])
    hmix = din("hmix", [128, 2])
    pdiv0 = din("pdiv0", [128, 64])
    poth0 = din("poth0", [NT, 128, 256])
    dmask_p = din("dmask_p", [128, 256]); hmix_p = din("hmix_p", [128, 2]); pdiv0_p = din("pdiv0_p", [128, 64])
    ROLE = {False: dict(xown=xown, xoth=xoth, cs_own=cs_own, cs_oth=cs_oth, dmask=dmask, hmix=hmix, pdiv0=pdiv0),
            True: dict(xown=xoth, xoth=xown, cs_own=cs_oth, cs_oth=cs_own, dmask=dmask_p, hmix=hmix_p, pdiv0=pdiv0_p)}
    AL = {}
    for nm, shp in (("w_in", [D, 3912]), ("pool_w", [4, 128, 128]), ("pool_scale", [512]), ("w_up_attn", [512, D]),
                    ("w_up_pool", [512, D]), ("w_out", [D, D]), ("ln1_g", [D]), ("ln1_b", [D]), ("router_w", [D, 32]),
                    ("router_b", [32]), ("exp_w_gu", [NEXP, D, 2048]), ("exp_b_gu", [NEXP, 2048]),
                    ("exp_w_down", [NEXP, D, D]), ("exp_b_down", [NEXP, D]), ("ple_w_gate", [D, D]),
                    ("ple_w_proj", [256, D]), ("ln2_g", [D]), ("ln2_b", [D])):
        if nm in ("exp_w_gu", "exp_w_down"):
            AL[nm] = [din("%s_%d" % (nm, i), shp) for i in range(2)]
        else:
            AL[nm] = din(nm, [2] + shp)
    ln0_g = din("ln0_g", [D]); ln0_b = din("ln0_b", [D])
    yout = dout("yout", [NT, 128, D])
    dbg_out = {}
    xo1 = nc.dram_tensor("xo1", [NT * 128, D], BF16).ap()
    bxo1 = Buf("xo1")
    Xs = nc.dram_tensor("Xs", [NEXP * CAP + 512, D], BF16).ap()
    Ys = nc.dram_tensor("Ys", [NEXP * CAP, D], F32).ap()
    bXs = Buf("Xs"); bYs = Buf("Ys")

    top = ExitStack()

    uniq = [0]

    def sb(es, name, shape, dt=F32):
        uniq[0] += 1
        name = "%s_%d" % (name, uniq[0])
        return T(es.enter_context(nc.sbuf_tensor(name, list(shape), dt)), name)

    def ps(es, name, shape, dt=F32):
        return T(es.enter_context(nc.psum_tensor(name, list(shape), dt)), name, excl=True)

    def bcast_row(vec_ap, n):
        return vec_ap.partition_broadcast(128)

    PB = [ps(top, "pb%d" % i, [128, 512]) for i in range(7)]
    PT = ps(top, "pt", [128, 8, 128], BF16)

    iot = sb(top, "iot", [128, 128]); pidx = sb(top, "pidx", [128, 1])
    identf = sb(top, "identf", [128, 128]); identb = sb(top, "identb", [128, 128], BF16)
    ones_b = sb(top, "ones_b", [128, 128], BF16)
    K.op("pool", lambda e: e.iota(iot[:], pattern=[[1, 128]], base=0, channel_multiplier=0,
                                  allow_small_or_imprecise_dtypes=True), writes=[iot.b])
    K.op("pool", lambda e: e.iota(pidx[:], pattern=[[0, 1]], base=0, channel_multiplier=1,
                                  allow_small_or_imprecise_dtypes=True), writes=[pidx.b])
    K.op("dve", lambda e: e.tensor_scalar(out=identf[:], in0=iot[:], scalar1=pidx[:, 0:1], scalar2=None,
                                          op0=ALU.is_equal), reads=[iot.b, pidx.b], writes=[identf.b])
    K.op("dve", lambda e: e.tensor_copy(out=identb[:], in_=identf[:]), reads=[identf.b], writes=[identb.b])
    K.op("dve", lambda e: e.memset(ones_b[:], 1.0), writes=[ones_b.b])

    xres = [sb(top, "xres%d" % j, [128, D]) for j in range(NT)]
    gvec = sb(top, "gvec", [128, D]); bvec = sb(top, "bvec", [128, D])

    def load_ln(g_ap, b_ap):
        K.dma("sp", lambda e: e.dma_start(out=gvec[:], in_=g_ap.partition_broadcast(128)), writes=[gvec.b])
        K.dma("sp", lambda e: e.dma_start(out=bvec[:], in_=b_ap.partition_broadcast(128)), writes=[bvec.b])

    lnst = sb(top, "lnst", [128, 2, 6]); lnmv = sb(top, "lnmv", [128, 2]); lnr = sb(top, "lnr", [128, 1])

    def layer_norm(xt):
        for h in range(2):
            K.op("dve", lambda e, h=h: e.bn_stats(out=lnst[:, h, :], in_=xt[:, h * 512:(h + 1) * 512]),
                 reads=[xt.b], writes=[lnst.b])
        K.op("dve", lambda e: e.bn_aggr(out=lnmv[:], in_=lnst[:]), reads=[lnst.b], writes=[lnmv.b])
        K.op("dve", lambda e: e.tensor_scalar(out=lnr[:], in0=lnmv[:, 1:2], scalar1=EPS, scalar2=None,
                                              op0=ALU.add), reads=[lnmv.b], writes=[lnr.b])
        K.op("act", lambda e: e.activation(out=lnr[:], in_=lnr[:], func=AF.Sqrt), reads=[lnr.b], writes=[lnr.b])
        K.op("dve", lambda e: e.reciprocal(out=lnr[:], in_=lnr[:]), reads=[lnr.b], writes=[lnr.b])
        K.op("dve", lambda e: e.tensor_scalar(out=xt[:], in0=xt[:], scalar1=lnmv[:, 0:1], scalar2=lnr[:, 0:1],
                                              op0=ALU.subtract, op1=ALU.mult), reads=[xt.b, lnmv.b, lnr.b], writes=[xt.b])
        K.op("dve", lambda e: e.tensor_tensor(out=xt[:], in0=xt[:], in1=gvec[:], op=ALU.mult),
             reads=[xt.b, gvec.b], writes=[xt.b])
        K.op("dve", lambda e: e.tensor_tensor(out=xt[:], in0=xt[:], in1=bvec[:], op=ALU.add),
             reads=[xt.b, bvec.b], writes=[xt.b])

    def transpose_x(xt, xT, xTf=None):
        for half in range(2):
            pb = PB[half]
            for k in range(4):
                kk = half * 4 + k
                K.op("pe", lambda e, pb=pb, k=k, kk=kk: e.transpose(out=pb[:, k * 128:(k + 1) * 128],
                                                                    in_=xt[:, kk * 128:(kk + 1) * 128], identity=identf[:]),
                     reads=[xt.b, identf.b], writes=[pb.b])
            K.op("act", lambda e, pb=pb, half=half: e.activation(
                out=xT[:, half * 4:(half + 1) * 4, :], in_=pb[:].rearrange("p (k t) -> p k t", k=4), func=AF.Copy),
                reads=[pb.b], writes=[xT.b])
            if xTf is not None:
                K.op("dve", lambda e, pb=pb, half=half: e.tensor_copy(
                    out=xTf[:, half * 4:(half + 1) * 4, :], in_=pb[:].rearrange("p (k t) -> p k t", k=4)),
                    reads=[pb.b], writes=[xTf.b])

    def rope(src, dst, cst, nh):
        sv, dv, sbuf, dbuf = src
        cos = cst[:, 0, 0:nh, :]
        sin = cst[:, 1, 0:nh, :]
        x1 = sv[:, :, 0:8]; x2 = sv[:, :, 8:16]
        if 'rope_act' not in SKIP: K.op("act", lambda e: e.activation(out=dv, in_=sv, func=AF.Copy), reads=[sbuf], writes=[dbuf])
        t = dst
        if 'rope_dve' in SKIP: return
        K.op("dve", lambda e: e.tensor_tensor(out=t[:, 0, 0:nh, :], in0=x1, in1=cos, op=ALU.mult), reads=[sbuf, cst.b], writes=[t.b])
        K.op("dve", lambda e: e.tensor_tensor(out=t[:, 1, 0:nh, :], in0=x2, in1=sin, op=ALU.mult), reads=[sbuf, cst.b], writes=[t.b])
        K.op("dve", lambda e: e.tensor_tensor(out=t[:, 2, 0:nh, :], in0=x2, in1=cos, op=ALU.mult), reads=[sbuf, cst.b], writes=[t.b])
        K.op("dve", lambda e: e.tensor_tensor(out=t[:, 3, 0:nh, :], in0=x1, in1=sin, op=ALU.mult), reads=[sbuf, cst.b], writes=[t.b])
        K.op("dve", lambda e: e.tensor_tensor(out=dv[:, :, 0:8], in0=t[:, 0, 0:nh, :], in1=t[:, 1, 0:nh, :], op=ALU.subtract),
             reads=[t.b], writes=[dbuf])
        K.op("dve", lambda e: e.tensor_tensor(out=dv[:, :, 8:16], in0=t[:, 2, 0:nh, :], in1=t[:, 3, 0:nh, :], op=ALU.add),
             reads=[t.b], writes=[dbuf])

    ropet = sb(top, "ropet", [128, 4, 8, 8])

    slots = sb(top, "slots", [128, NT, 4], I32); gates = sb(top, "gates", [128, NT, 4])
    class V:
        def __init__(self, ap, b):
            self.ap = ap; self.b = b

        def __getitem__(self, k):
            return self.ap[k]

    def run_layer(l, partner=False, zero_slabs=False):
        first = (l == 0)
        fused_in = (l == 1)
        last = (l == 1)
        R = ROLE[partner]
        xown = R["xown"]; xoth = R["xoth"]; cs_own = R["cs_own"]; cs_oth = R["cs_oth"]
        dmask = R["dmask"]; hmix = R["hmix"]; pdiv0 = R["pdiv0"]
        pown = poth0 if partner else pown_all[l]
        w_in = AL["w_in"][l]; pool_w = AL["pool_w"][l]; pool_scale = AL["pool_scale"][l]
        w_up_attn = AL["w_up_attn"][l]; w_up_pool = AL["w_up_pool"][l]; w_out = AL["w_out"][l]
        ln1_g = AL["ln1_g"][l]; ln1_b = AL["ln1_b"][l]; router_w = AL["router_w"][l]; router_b = AL["router_b"][l]
        exp_w_gu = AL["exp_w_gu"][l]; exp_b_gu = AL["exp_b_gu"][l]; exp_w_down = AL["exp_w_down"][l]; exp_b_down = AL["exp_b_down"][l]
        ple_w_gate = AL["ple_w_gate"][l]; ple_w_proj = AL["ple_w_proj"][l]; ln2_g = AL["ln2_g"][l]; ln2_b = AL["ln2_b"][l]
        esY = ExitStack()
        yaT = sb(esY, "yaT", [128, 4, NT * 128], BF16)
        uh = sb(esY, "uh", [128, NT, 4, 16])
        esAB = ExitStack()
        kT = sb(esAB, "kT", [128, 2, 2 * NT * 128], BF16)
        kiT = sb(esAB, "kiT", [128, 2 * NT * 128], BF16)
        K.op("dve", lambda e: e.memset(kT[64:128, :, :], 0.0), writes=[kT.b])
        K.op("dve", lambda e: e.memset(kiT[64:128, :], 0.0), writes=[kiT.b])
        vaug = sb(esAB, "vaug", [128, 2 * NT, 2, 65], BF16)
        K.op("dve", lambda e: e.memset(vaug[:], 1.0), writes=[vaug.b])

        if first:
            load_ln(ln0_g, ln0_b)

        esA = ExitStack()
        Wkv = sb(esA, "Wkv", [128, 8, 320], BF16)
        Wu_a = sb(esA, "Wu_a", [128, 8, 512], BF16)
        xtmps = [sb(esA, "xtmp%d" % i, [128, D]) for i in range(2)]
        xTa = [sb(esA, "xTa%d" % i, [128, 8, 128], BF16) for i in range(2)]
        kvbs = [sb(esA, "kvb%d" % i, [128, 5, 64], BF16) for i in range(2)]
        csas = [sb(esA, "csa%d" % i, [128, 2, 8, 8]) for i in range(2)]
        ropets = [sb(esA, "ropetA%d" % i, [128, 4, 8, 8]) for i in range(2)]
        if fused_in:
            xbm = sb(esA, "xbm", [128, D], BF16)

        def wslice(a, b):
            return w_in[:, a:b].rearrange("(k p) n -> p k n", p=128)

        zt = sb(esA, "zt", [128, 8, D], BF16)
        K.op("dve", lambda e: e.memset(zt[:], 0.0), writes=[zt.b])
        nrows = NEXP * CAP + 512
        for r0 in (range(0, nrows, 1024) if zero_slabs else ()):
            nb_ = min(1024, nrows - r0) // 128
            K.dma("sp", lambda e, r0=r0, nb_=nb_: e.dma_start(out=Xs[r0:r0 + nb_ * 128, :].rearrange("(a p) n -> p a n", p=128), in_=zt[:, 0:nb_, :]),
                  reads=[zt.b], writes=[bXs])

        K.dma("pool", lambda e: e.dma_start(out=Wkv[:, :, 0:128], in_=wslice(512, 640)), writes=[Wkv.b])
        K.dma("pool", lambda e: e.dma_start(out=Wkv[:, :, 128:192], in_=wslice(1280, 1344)), writes=[Wkv.b])
        K.dma("pool", lambda e: e.dma_start(out=Wkv[:, :, 192:320], in_=wslice(640, 768)), writes=[Wkv.b])
        K.dma("pool", lambda e: e.dma_start(out=Wu_a[:], in_=wslice(1352, 1864)), writes=[Wu_a.b])

        it = 0
        for j in range(nq):
            for s in range(2):
                kt = 2 * j + s
                src = xown if s == 0 else xoth
                cs_src = cs_own if s == 0 else cs_oth
                xt = xres[j] if s == 0 else xtmps[j % 2]
                kvb = kvbs[it % 2]; csa = csas[it % 2]; ropet_a = ropets[it % 2]; pbA = PB[2 + (it % 2)]
                xT = xTa[it % 2]; it += 1
                K.dma("sp", lambda e, cs_src=cs_src, j=j, csa=csa: e.dma_start(out=csa[:].rearrange("p a h d -> p (a h d)"), in_=cs_src[j]),
                      writes=[csa.b])
                if not fused_in:
                    K.dma("sp", lambda e, xt=xt, src=src, j=j: e.dma_start(out=xt[:], in_=src[j]), writes=[xt.b])
                    if first:
                        layer_norm(xt)
                    transpose_x(xt, xT)
                elif s == 0:
                    transpose_x(xt, xT)
                else:
                    K.dma("sp", lambda e, j=j: e.dma_start(out=xbm[:], in_=xo1[j * 128:(j + 1) * 128, :]), reads=[bxo1], writes=[xbm.b])
                    for k in range(8):
                        K.op("pe", lambda e, k=k: e.transpose(out=PT[:, k, :], in_=xbm[:, k * 128:(k + 1) * 128], identity=identb[:]),
                             reads=[xbm.b, identb.b], writes=[PT.b])
                    K.op("act", lambda e, xT=xT: e.activation(out=xT[:], in_=PT[:], func=AF.Copy), reads=[PT.b], writes=[xT.b])
                pb = pbA
                for k in range(8):
                    K.op("pe", lambda e, pb=pb, xT=xT, k=k: e.matmul(pb[:, 0:320], lhsT=xT[:, k, :], rhs=Wkv[:, k, :],
                                                                     start=(k == 0), stop=(k == 7)),
                         reads=[xT.b, Wkv.b], writes=[pb.b])
                if s == 1 and 'uh' not in SKIP:
                    for g in range(4):
                        for k in range(8):
                            K.op("pe", lambda e, pb=pb, xT=xT, k=k, g=g: e.matmul(
                                pb[:, 384 + g * 16:384 + (g + 1) * 16], lhsT=Wu_a[:, k, g * 128:(g + 1) * 128],
                                rhs=xT[:, k, 112:128], start=(k == 0), stop=(k == 7)),
                                reads=[xT.b, Wu_a.b], writes=[pb.b])
                    K.op("act", lambda e, pb=pb, j=j: e.activation(out=uh[:, j, :, :], in_=pb[:, 384:448].rearrange("p (g t) -> p g t", g=4),
                                                                   func=AF.Copy), reads=[pb.b], writes=[uh.b])
                if 'vaug' not in SKIP: K.op("act", lambda e, pb=pb, kt=kt: e.activation(out=vaug[:, kt, :, 0:64],
                                                                in_=pb[:, 192:320].rearrange("p (c d) -> p c d", c=2), func=AF.Copy),
                     reads=[pb.b], writes=[vaug.b])
                rope((pb[:, 0:192].rearrange("p (h d) -> p h d", h=3), kvb[:, 0:3, :], pb.b, kvb.b), ropet_a, csa, 3)
                for h in range(3):
                    K.op("pe", lambda e, h=h, kvb=kvb: e.transpose(out=PT[0:64, h, :], in_=kvb[:, h, :], identity=identb[:]),
                         reads=[kvb.b, identb.b], writes=[PT.b])
                K.op("act", lambda e, kt=kt: e.activation(out=kT[0:64, :, kt * 128:(kt + 1) * 128], in_=PT[0:64, 0:2, :], func=AF.Copy),
                     reads=[PT.b], writes=[kT.b])
                K.op("dve", lambda e, kt=kt: e.tensor_copy(out=kiT[0:64, kt * 128:(kt + 1) * 128], in_=PT[0:64, 2, :]),
                     reads=[PT.b], writes=[kiT.b])
        K.barrier()
        if stop_after == "A":
            dk = dout("d_kT", [64, 2 * NT * 128], BF16)
            K.dma("sp", lambda e: e.dma_start(out=dk, in_=kiT[:]), reads=[kiT.b])
            K.barrier()
            K.emit()
            K.close()
            return nc
        esA.close()

        esB = ExitStack()
        Wqq = sb(esB, "Wqq", [128, 8, 1032], BF16)
        K.dma("pool", lambda e: e.dma_start(out=Wqq[:, :, 0:512], in_=wslice(0, 512)), writes=[Wqq.b])
        K.dma("pool", lambda e: e.dma_start(out=Wqq[:, :, 512:1024], in_=wslice(768, 1280)), writes=[Wqq.b])
        K.dma("pool", lambda e: e.dma_start(out=Wqq[:, :, 1024:1032], in_=wslice(1344, 1352)), writes=[Wqq.b])
        order = []
        lo_, hi_ = 0, nq - 1
        while lo_ <= hi_:
            order.append(hi_); hi_ -= 1
            if lo_ <= hi_:
                order.append(lo_); lo_ += 1
        nks = [(2 * j_ + 2) * 128 for j_ in order]
        WS = max([nks[0]] + [nks[i] + nks[i + 1] for i in range(len(nks) - 1)])
        score_t = sb(esB, "score", [128, WS])
        sbufs = [Buf("scoreA"), Buf("scoreB")]
        mb_t = sb(esB, "mb", [128, WS], BF16)
        mbufs = [Buf("mbA"), Buf("mbB")]
        mbT = sb(esB, "mbT", [128, 2 * NT, 128], BF16)
        xTq = sb(esB, "xTq", [128, 8, 128], BF16)
        qb = sb(esB, "qb", [128, 8, 64], BF16); qib = sb(esB, "qib", [128, 8, 64], BF16)
        qTs = [sb(esB, "qT%d" % i, [128, 8, 128], BF16) for i in range(3)]; qiT = sb(esB, "qiT", [128, 8, 128], BF16)
        for t_ in (qTs[0], qTs[1], qTs[2], qiT):
            K.op("dve", lambda e, t_=t_: e.memset(t_[64:128, :, :], 0.0), writes=[t_.b])
        csq = sb(esB, "csq", [128, 2, 8, 8])
        wif = sb(esB, "wif", [128, 8]); absw = sb(esB, "absw", [128, 8]); sgn = sb(esB, "sgn", [128, 8])
        Dg = sb(esB, "Dg", [128, 8, 128], BF16)
        rbuf = [sb(esB, "rbuf%d" % i, [128, 512], BF16) for i in range(3)]
        PTs = [sb(esB, "PTs%d" % i, [128, 4, 128], BF16) for i in range(2)]
        dmk = sb(esB, "dmk", [128, 256])
        bmins = [sb(esB, "bmin%d" % i, [128, 8]) for i in range(2)]
        lo0s = [sb(esB, "lo0%d" % i, [128, 1]) for i in range(2)]; hi0s = [sb(esB, "hi0%d" % i, [128, 1]) for i in range(2)]
        wtab = sb(esB, "wtab", [128, NIT + 1]); p2 = sb(esB, "p2", [128, NIT + 1])
        mid = sb(esB, "mid", [128, 1]); cnt = sb(esB, "cnt", [128, 1]); tt = sb(esB, "tt", [128, 1])
        rng = sb(esB, "rng", [128, 1]); thr = sb(esB, "thr", [128, 1])
        rec = sb(esB, "rec", [128, 8]); yb = sb(esB, "yb", [128, 8, 64], BF16)
        K.dma("sp", lambda e: e.dma_start(out=dmk[:], in_=dmask), writes=[dmk.b])
        for n in range(NIT + 1):
            K.op("dve", lambda e, n=n: e.memset(p2[:, n:n + 1], 0.5 ** (n + 1)), writes=[p2.b])

        def stA(p_):
            j = order[p_]
            nkt = 2 * j + 2
            nk = nkt * 128
            off_ = 0 if p_ % 2 == 0 else WS - nk
            qT = qTs[p_ % 3]; bmin = bmins[p_ % 2]; lo0 = lo0s[p_ % 2]; hi0 = hi0s[p_ % 2]
            score = V(score_t[:, off_:off_ + nk], sbufs[p_ % 2]); mb = V(mb_t[:, off_:off_ + nk], mbufs[p_ % 2])
            K.dma("sp", lambda e, j=j: e.dma_start(out=csq[:].rearrange("p a h d -> p (a h d)"), in_=cs_own[j]), writes=[csq.b])
            transpose_x(xres[j], xTq)
            for blk, pb in ((0, PB[2]), (1, PB[3])):
                for k in range(8):
                    K.op("pe", lambda e, pb=pb, k=k, blk=blk: e.matmul(pb[:], lhsT=xTq[:, k, :], rhs=Wqq[:, k, blk * 512:(blk + 1) * 512],
                                                                       start=(k == 0), stop=(k == 7)),
                         reads=[xTq.b, Wqq.b], writes=[pb.b])
            for k in range(8):
                K.op("pe", lambda e, k=k: e.matmul(PB[4][:, 0:8], lhsT=xTq[:, k, :], rhs=Wqq[:, k, 1024:1032],
                                                   start=(k == 0), stop=(k == 7)),
                     reads=[xTq.b, Wqq.b], writes=[PB[4].b])
            rope((PB[2][:].rearrange("p (h d) -> p h d", h=8), qb[:], PB[2].b, qb.b), ropet, csq, 8)
            rope((PB[3][:].rearrange("p (h d) -> p h d", h=8), qib[:], PB[3].b, qib.b), ropet, csq, 8)
            K.op("act", lambda e: e.activation(out=wif[:], in_=PB[4][:, 0:8], func=AF.Copy), reads=[PB[4].b], writes=[wif.b])
            K.op("act", lambda e: e.activation(out=absw[:], in_=wif[:], func=AF.Abs, scale=IDX_SCALE), reads=[wif.b], writes=[absw.b])
            K.op("dve", lambda e: e.tensor_scalar(out=sgn[:], in0=wif[:], scalar1=0.0, scalar2=2.0,
                                                  op0=ALU.is_ge, op1=ALU.mult), reads=[wif.b], writes=[sgn.b])
            K.op("dve", lambda e: e.tensor_scalar(out=sgn[:], in0=sgn[:], scalar1=-1.0, scalar2=None,
                                                  op0=ALU.add), reads=[sgn.b], writes=[sgn.b])
            for h in range(8):
                K.op("dve", lambda e, h=h: e.tensor_scalar(out=Dg[:, h, :], in0=identb[:], scalar1=sgn[:, h:h + 1], scalar2=None,
                                                           op0=ALU.mult), reads=[identb.b, sgn.b], writes=[Dg.b])
            for h in range(8):
                K.op("pe", lambda e, h=h: e.transpose(out=PT[0:64, h, :], in_=qb[:, h, :], identity=identb[:]),
                     reads=[qb.b, identb.b], writes=[PT.b])
            K.op("act", lambda e: e.activation(out=qT[0:64, :, :], in_=PT[0:64, :, :], func=AF.Copy), reads=[PT.b], writes=[qT.b])
            for h in range(8):
                K.op("pe", lambda e, h=h: e.transpose(out=PT[0:64, h, :], in_=qib[:, h, :], identity=identb[:]),
                     reads=[qib.b, identb.b], writes=[PT.b])
            K.op("act", lambda e: e.activation(out=qiT[0:64, :, :], in_=PT[0:64, :, :], func=AF.Copy), reads=[PT.b], writes=[qiT.b])

            nblk = (nk + 511) // 512
            steps = [(blk, h) for blk in range(nblk) for h in range(8)]

            def dots(i):
                blk, h = steps[i]
                c0 = blk * 512; w = min(512, nk - c0)
                pd = PB[5 + (i % 2)]
                K.op("pe", lambda e: e.matmul(pd[:, 0:w], lhsT=qiT[:, h, :], rhs=kiT[:, c0:c0 + w], start=True, stop=True),
                     reads=[qiT.b, kiT.b], writes=[pd.b])

            def relu_acc(i):
                blk, h = steps[i]
                c0 = blk * 512; w = min(512, nk - c0)
                pd = PB[5 + (i % 2)]; rb = rbuf[i % 3]
                K.op("act", lambda e: e.activation(out=rb[:, 0:w], in_=pd[:, 0:w], func=AF.Relu, scale=absw[:, h:h + 1]),
                     reads=[pd.b, absw.b], writes=[rb.b])
                K.op("pe", lambda e: e.matmul(PB[4][:, 0:w], lhsT=Dg[:, h, :], rhs=rb[:, 0:w], start=(h == 0), stop=(h == 7)),
                     reads=[Dg.b, rb.b], writes=[PB[4].b])
                if h == 7:
                    K.op("dve", lambda e: e.tensor_scalar(out=score[:, c0:c0 + w], in0=PB[4][:, 0:w], scalar1=0.0, scalar2=3.0e38,
                                                          op0=ALU.add, op1=ALU.min, accum_out=bmin[:, blk:blk + 1]),
                         reads=[PB[4].b], writes=[score.b, bmin.b])

            for i in range(len(steps) + 1):
                if i < len(steps):
                    dots(i)
                if i >= 1:
                    relu_acc(i - 1)
            K.op("dve", lambda e, nblk=nblk: e.tensor_reduce(out=lo0[:], in_=bmin[:, 0:nblk], axis=AX.X, op=ALU.min),
                 reads=[bmin.b], writes=[lo0.b])
            K.op("dve", lambda e, nk=nk: e.tensor_tensor(out=score[:, nk - 256:nk], in0=score[:, nk - 256:nk], in1=dmk[:], op=ALU.add),
                 reads=[score.b, dmk.b], writes=[score.b])
            K.op("dve", lambda e, nk=nk: e.tensor_reduce(out=hi0[:], in_=score[:, 0:nk], axis=AX.X, op=ALU.max),
                 reads=[score.b], writes=[hi0.b])

        def stB(p_):
            j = order[p_]
            nkt = 2 * j + 2
            nk = nkt * 128
            off_ = 0 if p_ % 2 == 0 else WS - nk
            qT = qTs[p_ % 3]; bmin = bmins[p_ % 2]; lo0 = lo0s[p_ % 2]; hi0 = hi0s[p_ % 2]
            score = V(score_t[:, off_:off_ + nk], sbufs[p_ % 2]); mb = V(mb_t[:, off_:off_ + nk], mbufs[p_ % 2])
            K.op("dve", lambda e: e.tensor_tensor(out=rng[:], in0=hi0[:], in1=lo0[:], op=ALU.subtract), reads=[hi0.b, lo0.b], writes=[rng.b])
            K.op("dve", lambda e: e.tensor_scalar(out=rng[:], in0=rng[:], scalar1=1.0001, scalar2=1e-20, op0=ALU.mult, op1=ALU.add),
                 reads=[rng.b], writes=[rng.b])
            K.op("dve", lambda e: e.tensor_scalar(out=wtab[:], in0=p2[:], scalar1=rng[:, 0:1], scalar2=None, op0=ALU.mult),
                 reads=[p2.b, rng.b], writes=[wtab.b])
            K.op("dve", lambda e: e.tensor_tensor(out=mid[:], in0=lo0[:], in1=wtab[:, 0:1], op=ALU.add), reads=[lo0.b, wtab.b], writes=[mid.b])
            for n in range(NIT):
                K.op("dve", lambda e, nk=nk: e.tensor_scalar(out=mb[:, 0:nk], in0=score[:, 0:nk], scalar1=mid[:, 0:1], scalar2=0.0,
                                                             op0=ALU.is_ge, op1=ALU.add, accum_out=cnt[:]),
                     reads=[score.b, mid.b], writes=[mb.b, cnt.b])
                K.op("dve", lambda e: e.tensor_scalar(out=tt[:], in0=cnt[:], scalar1=255.5, scalar2=0.5, op0=ALU.is_ge, op1=ALU.subtract),
                     reads=[cnt.b], writes=[tt.b])
                K.op("dve", lambda e, n=n: e.scalar_tensor_tensor(out=mid[:], in0=tt[:], scalar=wtab[:, n:n + 1], in1=mid[:],
                                                                  op0=ALU.mult, op1=ALU.add),
                     reads=[tt.b, wtab.b, mid.b], writes=[mid.b])
            K.op("dve", lambda e: e.tensor_tensor(out=thr[:], in0=mid[:], in1=wtab[:, NIT:NIT + 1], op=ALU.subtract),
                 reads=[mid.b, wtab.b], writes=[thr.b])
            if "score" in dbg:
                K.dma("sp", lambda e, j=j, nk=nk: e.dma_start(out=dbg_out["score"][j][:, 0:nk], in_=score[:, 0:nk]), reads=[score.b])
                K.dma("sp", lambda e, j=j: e.dma_start(out=dbg_out["thr"][j][:, 0:1], in_=thr[:], allow_slow_non_contiguous=True), reads=[thr.b])
                K.dma("sp", lambda e, j=j: e.dma_start(out=dbg_out["thr"][j][:, 1:2], in_=cnt[:], allow_slow_non_contiguous=True), reads=[cnt.b])
            K.op("dve", lambda e, nk=nk: e.tensor_scalar(out=mb[:, 0:nk], in0=score[:, 0:nk], scalar1=thr[:, 0:1], scalar2=MASKV,
                                                         op0=ALU.is_lt, op1=ALU.mult), reads=[score.b, thr.b], writes=[mb.b])

        def stC(p_):
            j = order[p_]
            nkt = 2 * j + 2
            nk = nkt * 128
            off_ = 0 if p_ % 2 == 0 else WS - nk
            qT = qTs[p_ % 3]; bmin = bmins[p_ % 2]; lo0 = lo0s[p_ % 2]; hi0 = hi0s[p_ % 2]
            score = V(score_t[:, off_:off_ + nk], sbufs[p_ % 2]); mb = V(mb_t[:, off_:off_ + nk], mbufs[p_ % 2])
            for g0 in range(0, nkt, 8):
                g1 = min(nkt, g0 + 8)
                for kt in range(g0, g1):
                    K.op("pe", lambda e, kt=kt, g0=g0: e.transpose(out=PT[:, kt - g0, :], in_=mb[:, kt * 128:(kt + 1) * 128], identity=identb[:]),
                         reads=[mb.b, identb.b], writes=[PT.b])
                K.op("act", lambda e, g0=g0, g1=g1: e.activation(out=mbT[:, g0:g1, :], in_=PT[:, 0:g1 - g0, :], func=AF.Copy),
                     reads=[PT.b], writes=[mbT.b])
            asteps = [(kt, c) for kt in range(nkt) for c in range(2)]

            def logits(i):
                kt, c = asteps[i]
                pl = PB[5 + (i % 2)]; pts = PTs[i % 2]
                K.op("pe", lambda e: e.matmul(pl[:], lhsT=kT[:, c, kt * 128:(kt + 1) * 128], rhs=qT[:, 4 * c:4 * c + 4, :], start=True, stop=False),
                     reads=[kT.b, qT.b], writes=[pl.b])
                K.op("pe", lambda e: e.matmul(pl[:].rearrange("p (h q) -> p h q", h=4), lhsT=identb[:],
                                              rhs=mbT[:, kt, :].unsqueeze(1).to_broadcast([128, 4, 128]), start=False, stop=True),
                     reads=[identb.b, mbT.b], writes=[pl.b])
                K.op("act", lambda e: e.activation(out=pts[:].rearrange("p h q -> p (h q)"), in_=pl[:], func=AF.Exp, scale=0.125),
                     reads=[pl.b], writes=[pts.b])

            def pv(i):
                kt, c = asteps[i]
                pts = PTs[i % 2]; po = PB[2 + c]
                for hh in range(4):
                    K.op("pe", lambda e, hh=hh: e.matmul(po[:, hh * 65:(hh + 1) * 65], lhsT=pts[:, hh, :], rhs=vaug[:, kt, c, :],
                                                         start=(kt == 0 and hh == 0), stop=(kt == nkt - 1 and hh == 3), skip_group_check=True),
                         reads=[pts.b, vaug.b], writes=[po.b])

            for i in range(len(asteps) + 1):
                if i < len(asteps):
                    logits(i)
                if i >= 1:
                    pv(i - 1)
            for c in range(2):
                po = PB[2 + c]
                pov = po[:, 0:260].rearrange("p (h d) -> p h d", h=4)
                K.op("dve", lambda e, pov=pov, c=c: e.reciprocal(out=rec[:, 4 * c:4 * c + 4], in_=pov[:, :, 64]),
                     reads=[po.b], writes=[rec.b])
                for hh in range(4):
                    K.op("dve", lambda e, pov=pov, c=c, hh=hh: e.tensor_scalar(out=yb[:, 4 * c + hh, :], in0=pov[:, hh, 0:64],
                                                                               scalar1=rec[:, 4 * c + hh:4 * c + hh + 1], scalar2=None,
                                                                               op0=ALU.mult), reads=[po.b, rec.b], writes=[yb.b])
            if "yattn" in dbg:
                ydbg = sb(esB, "ydbg%d" % j, [128, 512])
                K.op("dve", lambda e, ydbg=ydbg: e.tensor_copy(out=ydbg[:], in_=yb[:].rearrange("p h d -> p (h d)")), reads=[yb.b], writes=[ydbg.b])
                K.dma("sp", lambda e, j=j, ydbg=ydbg: e.dma_start(out=dbg_out["yattn"][j], in_=ydbg[:]), reads=[ydbg.b])
            for m in range(4):
                K.op("pe", lambda e, m=m: e.transpose(out=PT[:, m, :], in_=yb[:, 2 * m:2 * m + 2, :].rearrange("p h d -> p (h d)"),
                                                      identity=identb[:]), reads=[yb.b, identb.b], writes=[PT.b])
            K.op("act", lambda e, j=j: e.activation(out=yaT[:, :, j * 128:(j + 1) * 128], in_=PT[:, 0:4, :], func=AF.Copy),
                 reads=[PT.b], writes=[yaT.b])

        for t in range(nq + 2):
            if t < nq:
                stA(t)
            if 1 <= t <= nq:
                stB(t - 1)
            if t >= 2:
                stC(t - 2)
        K.barrier()
        esB.close()
        esAB.close()

        if stop_after == "B":
            K.barrier()
            K.emit()
            K.close()
            top.close()
            return nc

        esC = ExitStack()
        Wu = sb(esC, "Wu", [128, 8, 512], BF16)
        Wga = sb(esC, "Wga", [128, 8, 1024], BF16); Wgp = sb(esC, "Wgp", [128, 8, 1024], BF16)
        Wua = sb(esC, "Wua", [128, 4, 1024], BF16); Wup = sb(esC, "Wup", [128, 4, 1024], BF16)
        Wo = sb(esC, "Wo", [128, 8, 1024], BF16)
        Wpl = sb(esC, "Wpl", [128, 4, 128], BF16)
        psc = sb(esC, "psc", [128, 4]); hm = sb(esC, "hm", [128, 2]); pdv = sb(esC, "pdv", [128, 4, 16])
        K.dma("pool", lambda e: e.dma_start(out=Wu[:], in_=wslice(1352, 1864)), writes=[Wu.b])
        K.dma("pool", lambda e: e.dma_start(out=Wga[:], in_=wslice(1864, 2888)), writes=[Wga.b])
        K.dma("pool", lambda e: e.dma_start(out=Wgp[:], in_=wslice(2888, 3912)), writes=[Wgp.b])
        K.dma("pool", lambda e: e.dma_start(out=Wua[:], in_=w_up_attn.rearrange("(k p) n -> p k n", p=128)), writes=[Wua.b])
        K.dma("pool", lambda e: e.dma_start(out=Wup[:], in_=w_up_pool.rearrange("(k p) n -> p k n", p=128)), writes=[Wup.b])
        K.dma("pool", lambda e: e.dma_start(out=Wo[:], in_=w_out.rearrange("(k p) n -> p k n", p=128)), writes=[Wo.b])
        K.dma("pool", lambda e: e.dma_start(out=Wpl[:], in_=pool_w.rearrange("g c d -> c g d")), writes=[Wpl.b])
        K.dma("sp", lambda e: e.dma_start(out=psc[:], in_=pool_scale.rearrange("(g p) -> p g", p=128), allow_slow_non_contiguous=True), writes=[psc.b])
        K.dma("sp", lambda e: e.dma_start(out=hm[:], in_=hmix), writes=[hm.b])
        K.dma("sp", lambda e: e.dma_start(out=pdv[:].rearrange("p g t -> p (g t)"), in_=pdiv0), writes=[pdv.b])
        load_ln(ln1_g, ln1_b)
        TG = min(4, nq)
        NTK = TG * 128
        xTc = sb(esC, "xTc", [128, 8, NTK], BF16)
        uT = sb(esC, "uT", [128, 4, 144]); T2 = sb(esC, "T2", [128, 4, 144]); T4 = sb(esC, "T4", [128, 4, 144])
        T8 = sb(esC, "T8", [128, 4, 144]); T16 = sb(esC, "T16", [128, 1, 144])
        htmp = sb(esC, "htmp", [128, 4, 16]); dtl = sb(esC, "dtl", [128, 4, NTK], BF16); tmpd = sb(esC, "tmpd", [128, 4, 16])
        ypT = sb(esC, "ypT", [128, 4, NTK], BF16)
        sg = [sb(esC, "sg%d" % i, [128, NTK], BF16) for i in range(2)]
        pr = [sb(esC, "pr%d" % i, [128, NTK], BF16) for i in range(2)]
        mgT = sb(esC, "mgT", [128, 8, NTK], BF16)
        WIN = (2, 4, 8, 16)

        for j0 in range(0, nq, TG):
            for jj in range(TG):
                transpose_x(xres[j0 + jj], V(xTc[:, :, jj * 128:(jj + 1) * 128], xTc.b))
            for g in range(4):
                for k in range(8):
                    K.op("pe", lambda e, g=g, k=k: e.matmul(PB[2 + g][:, 0:NTK], lhsT=Wu[:, k, g * 128:(g + 1) * 128], rhs=xTc[:, k, :],
                                                            start=(k == 0), stop=(k == 7)), reads=[Wu.b, xTc.b], writes=[PB[2 + g].b])
            for jj in range(TG):
                j = j0 + jj
                ts_ = slice(jj * 128, (jj + 1) * 128)
                for g in range(4):
                    K.op("act", lambda e, g=g, ts_=ts_: e.activation(out=uT[:, g, 16:144], in_=PB[2 + g][:, ts_], func=AF.Copy),
                         reads=[PB[2 + g].b], writes=[uT.b])
                if j == 0:
                    K.op("dve", lambda e: e.tensor_scalar(out=uT[:, :, 0:16], in0=uh[:, 0, :, :], scalar1=hm[:, 1:2], scalar2=None, op0=ALU.mult),
                         reads=[uh.b, hm.b], writes=[uT.b])
                else:
                    K.op("dve", lambda e, j=j: e.tensor_scalar(out=htmp[:], in0=uh[:, j - 1, :, :], scalar1=hm[:, 0:1], scalar2=None, op0=ALU.mult),
                         reads=[uh.b, hm.b], writes=[htmp.b])
                    K.op("dve", lambda e, j=j: e.scalar_tensor_tensor(out=uT[:, :, 0:16], in0=uh[:, j, :, :], scalar=hm[:, 1:2], in1=htmp[:],
                                                                      op0=ALU.mult, op1=ALU.add), reads=[uh.b, hm.b, htmp.b], writes=[uT.b])
                K.op("dve", lambda e: e.tensor_tensor(out=T2[:, :, 1:144], in0=uT[:, :, 1:144], in1=uT[:, :, 0:143], op=ALU.add), reads=[uT.b], writes=[T2.b])
                K.op("dve", lambda e: e.tensor_tensor(out=T4[:, 1:4, 3:144], in0=T2[:, 1:4, 3:144], in1=T2[:, 1:4, 1:142], op=ALU.add), reads=[T2.b], writes=[T4.b])
                K.op("dve", lambda e: e.tensor_tensor(out=T8[:, 2:4, 7:144], in0=T4[:, 2:4, 7:144], in1=T4[:, 2:4, 3:140], op=ALU.add), reads=[T4.b], writes=[T8.b])
                K.op("dve", lambda e: e.tensor_tensor(out=T16[:, 0:1, 15:144], in0=T8[:, 3:4, 15:144], in1=T8[:, 3:4, 7:136], op=ALU.add), reads=[T8.b], writes=[T16.b])
                Sg = (T2, T4, T8, T16)
                Si = (0, 1, 2, 0)
                for g in range(4):
                    K.op("dve", lambda e, g=g, ts_=ts_: e.scalar_tensor_tensor(out=dtl[:, g, ts_], in0=Sg[g][:, Si[g], 16:144], scalar=1.0 / WIN[g], in1=uT[:, g, 16:144],
                                                                              op0=ALU.mult, op1=ALU.subtract), reads=[Sg[g].b, uT.b], writes=[dtl.b])
                if j == 0:
                    for g in range(4):
                        K.op("dve", lambda e, g=g: e.tensor_tensor(out=tmpd[:, g, :], in0=Sg[g][:, Si[g], 16:32], in1=pdv[:, g, :], op=ALU.mult),
                             reads=[Sg[g].b, pdv.b], writes=[tmpd.b])
                    K.op("dve", lambda e: e.tensor_tensor(out=dtl[:, :, 0:16], in0=tmpd[:], in1=uT[:, :, 16:32], op=ALU.subtract),
                         reads=[tmpd.b, uT.b], writes=[dtl.b])
            for g in range(4):
                pbp = PB[2 + (g % 2)]
                K.op("pe", lambda e, g=g, pbp=pbp: e.matmul(pbp[:, 0:NTK], lhsT=Wpl[:, g, :], rhs=dtl[:, g, :], start=True, stop=True),
                     reads=[Wpl.b, dtl.b], writes=[pbp.b])
                K.op("act", lambda e, g=g, pbp=pbp: e.activation(out=ypT[:, g, :], in_=pbp[:, 0:NTK], func=AF.Identity, scale=psc[:, g:g + 1]),
                     reads=[pbp.b, psc.b], writes=[ypT.b])
            bi = 0
            for m in range(8):
                ms = slice(m * 128, (m + 1) * 128)
                for br in range(2):
                    pu = PB[(bi % 3) * 2]; pg_ = PB[(bi % 3) * 2 + 1]
                    sgm = sg[bi % 2]; prm = pr[bi % 2]; bi += 1
                    Wup_ = Wua if br == 0 else Wup
                    Wg_ = Wga if br == 0 else Wgp
                    for kb in range(4):
                        rhs_t, rhs_b = ((yaT[:, kb, j0 * 128:j0 * 128 + NTK], yaT.b) if br == 0 else (ypT[:, kb, :], ypT.b))
                        K.op("pe", lambda e, pu=pu, kb=kb, ms=ms, Wup_=Wup_, rhs_t=rhs_t: e.matmul(pu[:, 0:NTK], lhsT=Wup_[:, kb, ms], rhs=rhs_t,
                                                                                                  start=(kb == 0), stop=(kb == 3)),
                             reads=[Wup_.b, rhs_b], writes=[pu.b])
                    for k in range(8):
                        K.op("pe", lambda e, pg_=pg_, k=k, ms=ms, Wg_=Wg_: e.matmul(pg_[:, 0:NTK], lhsT=Wg_[:, k, ms], rhs=xTc[:, k, :],
                                                                                   start=(k == 0), stop=(k == 7)), reads=[Wg_.b, xTc.b], writes=[pg_.b])
                    K.op("act", lambda e, pg_=pg_, sgm=sgm: e.activation(out=sgm[:], in_=pg_[:, 0:NTK], func=AF.Sigmoid), reads=[pg_.b], writes=[sgm.b])
                    if br == 0:
                        K.op("dve", lambda e, pu=pu, sgm=sgm, prm=prm: e.tensor_tensor(out=prm[:], in0=pu[:, 0:NTK], in1=sgm[:], op=ALU.mult),
                             reads=[pu.b, sgm.b], writes=[prm.b])
                        pr_a = prm
                    else:
                        K.op("dve", lambda e, pu=pu, sgm=sgm, prm=prm: e.tensor_tensor(out=prm[:], in0=pu[:, 0:NTK], in1=sgm[:], op=ALU.mult),
                             reads=[pu.b, sgm.b], writes=[prm.b])
                        K.op("dve", lambda e, prm=prm, pr_a=pr_a, m=m: e.tensor_tensor(out=mgT[:, m, :], in0=pr_a[:], in1=prm[:], op=ALU.add),
                             reads=[pr_a.b, prm.b], writes=[mgT.b])
            for jj in range(TG):
                j = j0 + jj
                for n in range(2):
                    pbo = PB[(jj * 2 + n) % 6]
                    for m in range(8):
                        K.op("pe", lambda e, pbo=pbo, m=m, n=n, jj=jj: e.matmul(pbo[:], lhsT=mgT[:, m, jj * 128:(jj + 1) * 128], rhs=Wo[:, m, n * 512:(n + 1) * 512],
                                                                               start=(m == 0), stop=(m == 7)), reads=[mgT.b, Wo.b], writes=[pbo.b])
                    K.op("dve", lambda e, pbo=pbo, n=n, j=j: e.scalar_tensor_tensor(out=xres[j][:, n * 512:(n + 1) * 512], in0=xres[j][:, n * 512:(n + 1) * 512],
                                                                                   scalar=ALPHA, in1=pbo[:], op0=ALU.mult, op1=ALU.add),
                         reads=[xres[j].b, pbo.b], writes=[xres[j].b])
                layer_norm(xres[j])
        K.barrier()
        esC.close()
        esY.close()
        if stop_after == "C":
            K.emit(); K.close(); top.close()
            return nc

        esD1 = ExitStack()
        Wpg = sb(esD1, "Wpg", [128, 8, 1024], BF16); Wpp = sb(esD1, "Wpp", [128, 2, 1024], BF16)
        Wr = sb(esD1, "Wr", [128, 8, 32]); rbr = sb(esD1, "rbr", [1, 32]); ones_f = sb(esD1, "ones_f", [1, 128])
        Ust = sb(esD1, "Ust", [128, 128], BF16); ecap = sb(esD1, "ecap", [128, 32]); base = sb(esD1, "base", [128, 32])
        zer = sb(esD1, "zer", [128, 32])
        K.dma("pool", lambda e: e.dma_start(out=Wpg[:], in_=ple_w_gate.rearrange("(k p) n -> p k n", p=128)), writes=[Wpg.b])
        K.dma("pool", lambda e: e.dma_start(out=Wpp[:], in_=ple_w_proj.rearrange("(k p) n -> p k n", p=128)), writes=[Wpp.b])
        K.dma("sp", lambda e: e.dma_start(out=Wr[:], in_=router_w.rearrange("(k p) n -> p k n", p=128)), writes=[Wr.b])
        K.dma("sp", lambda e: e.dma_start(out=rbr[:], in_=router_b.rearrange("(a n) -> a n", a=1)), writes=[rbr.b])
        K.op("dve", lambda e: e.memset(ones_f[:], 1.0), writes=[ones_f.b])
        K.op("dve", lambda e: e.memset(base[:], 0.0), writes=[base.b])
        K.op("dve", lambda e: e.memset(zer[:], 0.0), writes=[zer.b])
        K.op("dve", lambda e: e.tensor_scalar(out=Ust[:], in0=iot[:], scalar1=pidx[:, 0:1], scalar2=None, op0=ALU.is_gt),
             reads=[iot.b, pidx.b], writes=[Ust.b])
        K.op("pool", lambda e: e.iota(ecap[:], pattern=[[CAP, 32]], base=0, channel_multiplier=0, allow_small_or_imprecise_dtypes=True),
             writes=[ecap.b])
        x1T = sb(esD1, "x1T", [128, 8, 128], BF16); x1Tf = sb(esD1, "x1Tf", [128, 8, 128])
        x1b = [sb(esD1, "x1b%d" % i, [128, D], BF16) for i in range(2)]
        lg = sb(esD1, "lg", [128, 32]); top8 = sb(esD1, "top8", [128, 8]); msk = sb(esD1, "msk", [128, 32])
        mskb = sb(esD1, "mskb", [128, 32], BF16); nv1 = sb(esD1, "nv1", [128, 1]); ex = sb(esD1, "ex", [128, 32])
        den = sb(esD1, "den", [128, 1]); G = sb(esD1, "G", [128, 32]); slotf = sb(esD1, "slotf", [128, 32])
        incl = sb(esD1, "incl", [128, 32]); rank = sb(esD1, "rank", [128, 32]); selk = sb(esD1, "selk", [128, 32])
        tm32 = sb(esD1, "tm32", [128, 32]); sk4 = sb(esD1, "sk4", [128, 4])
        ptl = sb(esD1, "ptl", [128, 256]); pTb = sb(esD1, "pTb", [128, 2, 128], BF16); sgp = sb(esD1, "sgp", [128, D])
        for j in range(nq):
            xb_ = x1b[j % 2]
            transpose_x(xres[j], x1T, x1Tf)
            K.op("act", lambda e, xb_=xb_, j=j: e.activation(out=xb_[:], in_=xres[j][:], func=AF.Copy), reads=[xres[j].b], writes=[xb_.b])
            for k in range(8):
                K.op("pe", lambda e, k=k: e.matmul(PB[2][:, 0:32], lhsT=x1Tf[:, k, :], rhs=Wr[:, k, :], start=(k == 0), stop=False),
                     reads=[x1Tf.b, Wr.b], writes=[PB[2].b])
            K.op("pe", lambda e: e.matmul(PB[2][:, 0:32], lhsT=ones_f[0:1, :], rhs=rbr[0:1, :], start=False, stop=True),
                 reads=[ones_f.b, rbr.b], writes=[PB[2].b])
            K.op("dve", lambda e: e.tensor_copy(out=lg[:], in_=PB[2][:, 0:32]), reads=[PB[2].b], writes=[lg.b])
            K.op("dve", lambda e: e.max(out=top8[:], in_=lg[:]), reads=[lg.b], writes=[top8.b])
            K.op("dve", lambda e: e.tensor_scalar(out=msk[:], in0=lg[:], scalar1=top8[:, 3:4], scalar2=None, op0=ALU.is_ge), reads=[lg.b, top8.b], writes=[msk.b])
            K.op("dve", lambda e: e.tensor_copy(out=mskb[:], in_=msk[:]), reads=[msk.b], writes=[mskb.b])
            K.op("dve", lambda e: e.tensor_scalar(out=nv1[:], in0=top8[:, 0:1], scalar1=-1.0, scalar2=None, op0=ALU.mult), reads=[top8.b], writes=[nv1.b])
            K.op("act", lambda e: e.activation(out=ex[:], in_=lg[:], func=AF.Exp, bias=nv1[:, 0:1]), reads=[lg.b, nv1.b], writes=[ex.b])
            K.op("dve", lambda e: e.tensor_tensor(out=ex[:], in0=ex[:], in1=msk[:], op=ALU.mult), reads=[ex.b, msk.b], writes=[ex.b])
            K.op("dve", lambda e: e.tensor_reduce(out=den[:], in_=ex[:], axis=AX.X, op=ALU.add), reads=[ex.b], writes=[den.b])
            K.op("dve", lambda e: e.reciprocal(out=den[:], in_=den[:]), reads=[den.b], writes=[den.b])
            K.op("dve", lambda e: e.tensor_scalar(out=G[:], in0=ex[:], scalar1=den[:, 0:1], scalar2=None, op0=ALU.mult), reads=[ex.b, den.b], writes=[G.b])
            K.op("pe", lambda e: e.matmul(PB[3][:, 0:32], lhsT=Ust[:], rhs=mskb[:], start=True, stop=True), reads=[Ust.b, mskb.b], writes=[PB[3].b])
            K.op("pe", lambda e: e.matmul(PB[3][:, 32:64], lhsT=ones_b[:], rhs=mskb[:], start=True, stop=True), reads=[ones_b.b, mskb.b], writes=[PB[3].b])
            K.op("dve", lambda e: e.tensor_tensor(out=slotf[:], in0=PB[3][:, 0:32], in1=base[:], op=ALU.add), reads=[PB[3].b, base.b], writes=[slotf.b])
            K.op("dve", lambda e: e.tensor_tensor(out=slotf[:], in0=slotf[:], in1=ecap[:], op=ALU.add), reads=[slotf.b, ecap.b], writes=[slotf.b])
            K.op("dve", lambda e: e.tensor_tensor(out=base[:], in0=PB[3][:, 32:64], in1=base[:], op=ALU.add), reads=[PB[3].b, base.b], writes=[base.b])
            K.op("dve", lambda e: e.tensor_tensor_scan(out=incl[:], data0=msk[:], data1=zer[:], initial=0.0, op0=ALU.add, op1=ALU.add),
                 reads=[msk.b, zer.b], writes=[incl.b])
            K.op("dve", lambda e: e.tensor_tensor(out=rank[:], in0=incl[:], in1=msk[:], op=ALU.subtract), reads=[incl.b, msk.b], writes=[rank.b])
            for k4 in range(4):
                K.op("dve", lambda e, k4=k4: e.scalar_tensor_tensor(out=selk[:], in0=rank[:], scalar=float(k4), in1=msk[:], op0=ALU.is_equal, op1=ALU.mult),
                     reads=[rank.b, msk.b], writes=[selk.b])
                K.op("dve", lambda e: e.tensor_tensor(out=tm32[:], in0=selk[:], in1=slotf[:], op=ALU.mult), reads=[selk.b, slotf.b], writes=[tm32.b])
                K.op("dve", lambda e, k4=k4: e.tensor_reduce(out=sk4[:, k4:k4 + 1], in_=tm32[:], axis=AX.X, op=ALU.add), reads=[tm32.b], writes=[sk4.b])
                K.op("dve", lambda e: e.tensor_tensor(out=tm32[:], in0=selk[:], in1=G[:], op=ALU.mult), reads=[selk.b, G.b], writes=[tm32.b])
                K.op("dve", lambda e, k4=k4, j=j: e.tensor_reduce(out=gates[:, j, k4:k4 + 1], in_=tm32[:], axis=AX.X, op=ALU.add), reads=[tm32.b], writes=[gates.b])
            K.op("dve", lambda e, j=j: e.tensor_copy(out=slots[:, j, :], in_=sk4[:]), reads=[sk4.b], writes=[slots.b])
            for k4 in range(4):
                K.dma("pool", lambda e, k4=k4, j=j, xb_=xb_: e.indirect_dma_start(out=Xs, out_offset=bass.IndirectOffsetOnAxis(slots[:, j, k4:k4 + 1], 0),
                                                                                 in_=xb_[:], in_offset=None), reads=[xb_.b, slots.b], writes=[bXs])
            K.dma("sp", lambda e, j=j: e.dma_start(out=ptl[:], in_=pown[j]), writes=[ptl.b])
            for k in range(2):
                K.op("pe", lambda e, k=k: e.transpose(out=PB[4][:, k * 128:(k + 1) * 128], in_=ptl[:, k * 128:(k + 1) * 128], identity=identf[:]),
                     reads=[ptl.b, identf.b], writes=[PB[4].b])
            K.op("act", lambda e: e.activation(out=pTb[:], in_=PB[4][:, 0:256].rearrange("p (k t) -> p k t", k=2), func=AF.Copy), reads=[PB[4].b], writes=[pTb.b])
            for n in range(2):
                for k in range(8):
                    K.op("pe", lambda e, n=n, k=k: e.matmul(PB[5 + n][:], lhsT=x1T[:, k, :], rhs=Wpg[:, k, n * 512:(n + 1) * 512], start=(k == 0), stop=(k == 7)),
                         reads=[x1T.b, Wpg.b], writes=[PB[5 + n].b])
                K.op("act", lambda e, n=n: e.activation(out=sgp[:, n * 512:(n + 1) * 512], in_=PB[5 + n][:], func=AF.Sigmoid), reads=[PB[5 + n].b], writes=[sgp.b])
                for k in range(2):
                    K.op("pe", lambda e, n=n, k=k: e.matmul(PB[n][:], lhsT=pTb[:, k, :], rhs=Wpp[:, k, n * 512:(n + 1) * 512], start=(k == 0), stop=(k == 1)),
                         reads=[pTb.b, Wpp.b], writes=[PB[n].b])
                K.op("dve", lambda e, n=n: e.tensor_tensor(out=sgp[:, n * 512:(n + 1) * 512], in0=PB[n][:], in1=sgp[:, n * 512:(n + 1) * 512], op=ALU.mult),
                     reads=[PB[n].b, sgp.b], writes=[sgp.b])
            K.op("dve", lambda e, j=j: e.scalar_tensor_tensor(out=xres[j][:], in0=xres[j][:], scalar=ALPHA, in1=sgp[:], op0=ALU.mult, op1=ALU.add),
                 reads=[xres[j].b, sgp.b], writes=[xres[j].b])
        K.barrier()
        esD1.close()

        esD2 = ExitStack()
        NR = 16
        ring = [sb(esD2, "ring%d" % i, [128, 8, 256], BF16) for i in range(NR)]
        bgur = [sb(esD2, "bgur%d" % i, [16, 128]) for i in range(2)]
        bg = [sb(esD2, "bg%d" % i, [128, 16]) for i in range(2)]
        bu1 = [sb(esD2, "bu1%d" % i, [128, 8]) for i in range(2)]
        bdb = [sb(esD2, "bdb%d" % i, [1, D], BF16) for i in range(2)]
        xgs = [sb(esD2, "xg%d" % i, [128, 3, D], BF16) for i in range(2)]
        XsTs = [sb(esD2, "XsT%d" % i, [128, 8, CAP], BF16) for i in range(2)]
        actT = sb(esD2, "actT", [128, 8, CAP], BF16)
        gt = [sb(esD2, "gt%d" % i, [128, CAP]) for i in range(2)]; sgx = [sb(esD2, "sgx%d" % i, [128, CAP]) for i in range(2)]
        uA = [sb(esD2, "uA%d" % i, [128, CAP]) for i in range(2)]; gs = [sb(esD2, "gs%d" % i, [128, CAP]) for i in range(2)]
        ysb = [sb(esD2, "ysb%d" % i, [128, D]) for i in range(2)]
        nexp = NEXP
        ring_i = [0]
        yi = [0]
        UNITS = {}

        ORDER = [("g", 0), ("u", 0), ("g", 1), ("u", 1), ("g", 2), ("u", 2), ("g", 3), ("u", 3), ("d", 0), ("d", 1), ("d", 2), ("d", 3)]
        for ex_ in range(nexp):
            UNITS[ex_] = {kq: ring[(12 * ex_ + pos) % NR] for pos, kq in enumerate(ORDER)}
        nxt = [0]

        def issue_loads(n):
            for _ in range(n):
                g = nxt[0]
                if g >= 12 * nexp:
                    return
                nxt[0] += 1
                ex_, pos = divmod(g, 12)
                kind, q = ORDER[pos]
                r = ring[g % NR]
                if kind == "g":
                    src = exp_w_gu[ex_][:, q * 256:(q + 1) * 256]
                elif kind == "u":
                    src = exp_w_gu[ex_][:, 1024 + q * 256:1024 + (q + 1) * 256]
                else:
                    src = exp_w_down[ex_][:, q * 256:(q + 1) * 256]
                K.dma("pool", lambda e, r=r, src=src: e.dma_start(out=r[:], in_=src.rearrange("(k p) n -> p k n", p=128)), writes=[r.b])

        def load_bias(ex_):
            bdx = bdb[ex_ % 2]
            K.dma("pool", lambda e: e.dma_start(out=bdx[:], in_=exp_b_down[ex_].rearrange("(a n) -> a n", a=1)), writes=[bdx.b])

        def prep(ex_):
            xg = xgs[ex_ % 2]; XsT = XsTs[ex_ % 2]
            bgr = bgur[ex_ % 2]; bgx = bg[ex_ % 2]; bux = bu1[ex_ % 2]
            K.dma("sp", lambda e: e.dma_start(out=bgr[:], in_=exp_b_gu[ex_].rearrange("(i p) -> i p", p=128)), writes=[bgr.b])
            K.dma("sp", lambda e: e.dma_start(out=xg[:], in_=Xs[ex_ * CAP:(ex_ + 1) * CAP, :].rearrange("(b p) n -> p b n", p=128)),
                  reads=[bXs], writes=[xg.b])
            K.op("pe", lambda e: e.transpose(out=PB[6][:, 0:16], in_=bgr[0:16, :], identity=identf[0:16, 0:16]),
                 reads=[bgr.b, identf.b], writes=[PB[6].b])
            K.op("act", lambda e: e.activation(out=bgx[:], in_=PB[6][:, 0:16], func=AF.Copy), reads=[PB[6].b], writes=[bgx.b])
            K.op("dve", lambda e: e.tensor_scalar(out=bux[:], in0=bgx[:, 8:16], scalar1=1.0, scalar2=None, op0=ALU.add),
                 reads=[bgx.b], writes=[bux.b])
            for blk in range(3):
                for kc in range(8):
                    K.op("pe", lambda e, blk=blk, kc=kc: e.transpose(out=PT[:, kc, :], in_=xg[:, blk, kc * 128:(kc + 1) * 128], identity=identb[:]),
                         reads=[xg.b, identb.b], writes=[PT.b])
                K.op("act", lambda e, blk=blk: e.activation(out=XsT[:, :, blk * 128:(blk + 1) * 128], in_=PT[:], func=AF.Copy), reads=[PT.b], writes=[XsT.b])

        def gate_up(ex_):
            us = UNITS[ex_]; XsT = XsTs[ex_ % 2]; bgx = bg[ex_ % 2]; bux = bu1[ex_ % 2]
            for i in range(8):
                pgt = PB[(i % 2) * 2]; put = PB[(i % 2) * 2 + 1]
                gti = gt[i % 2]; sgi = sgx[i % 2]; uAi = uA[i % 2]; gsi = gs[i % 2]
                cs_ = slice((i % 2) * 128, (i % 2 + 1) * 128)
                rg = us[("g", i // 2)]; ru = us[("u", i // 2)]
                for kc in range(8):
                    K.op("pe", lambda e, kc=kc, pgt=pgt, rg=rg, cs_=cs_: e.matmul(pgt[:, 0:CAP], lhsT=rg[:, kc, cs_], rhs=XsT[:, kc, :], start=(kc == 0), stop=(kc == 7)),
                         reads=[rg.b, XsT.b], writes=[pgt.b])
                for kc in range(8):
                    K.op("pe", lambda e, kc=kc, put=put, ru=ru, cs_=cs_: e.matmul(put[:, 0:CAP], lhsT=ru[:, kc, cs_], rhs=XsT[:, kc, :], start=(kc == 0), stop=(kc == 7)),
                         reads=[ru.b, XsT.b], writes=[put.b])
                K.op("dve", lambda e, i=i, gti=gti, pgt=pgt: e.tensor_scalar(out=gti[:], in0=pgt[:, 0:CAP], scalar1=bgx[:, i:i + 1], scalar2=7.0, op0=ALU.add, op1=ALU.min),
                     reads=[pgt.b, bgx.b], writes=[gti.b])
                K.op("act", lambda e, sgi=sgi, gti=gti: e.activation(out=sgi[:], in_=gti[:], func=AF.Sigmoid, scale=1.702), reads=[gti.b], writes=[sgi.b])
                K.op("dve", lambda e, i=i, uAi=uAi, put=put: e.tensor_scalar(out=uAi[:], in0=put[:, 0:CAP], scalar1=bux[:, i:i + 1], scalar2=8.0, op0=ALU.add, op1=ALU.min),
                     reads=[put.b, bux.b], writes=[uAi.b])
                K.op("dve", lambda e, gsi=gsi, gti=gti, sgi=sgi: e.tensor_tensor(out=gsi[:], in0=gti[:], in1=sgi[:], op=ALU.mult), reads=[gti.b, sgi.b], writes=[gsi.b])
                K.op("dve", lambda e, i=i, uAi=uAi, gsi=gsi: e.scalar_tensor_tensor(out=actT[:, i, :], in0=uAi[:], scalar=-6.0, in1=gsi[:], op0=ALU.max, op1=ALU.mult),
                     reads=[uAi.b, gsi.b], writes=[actT.b])
                if i % 2 == 1:
                    issue_loads(2)

        def down(ex_):
            us = UNITS[ex_]; bdx = bdb[ex_ % 2]
            di = 0
            for rb_ in range(3):
                ys = ysb[yi[0] % 2]; yi[0] += 1
                for half in range(2):
                    pbd = PB[4 + (di % 2)]; di += 1
                    for qq in range(2):
                        r = us[("d", half * 2 + qq)]
                        c0_ = half * 512 + qq * 256
                        K.op("pe", lambda e, qq=qq, pbd=pbd, c0_=c0_: e.matmul(pbd[:, qq * 256:(qq + 1) * 256], lhsT=ones_b[0:1, :], rhs=bdx[0:1, c0_:c0_ + 256],
                                                                             start=True, stop=False), reads=[ones_b.b, bdx.b], writes=[pbd.b])
                        for i in range(8):
                            K.op("pe", lambda e, qq=qq, i=i, r=r, pbd=pbd, rb_=rb_: e.matmul(pbd[:, qq * 256:(qq + 1) * 256], lhsT=actT[:, i, rb_ * 128:(rb_ + 1) * 128], rhs=r[:, i, :],
                                                                                          start=False, stop=(i == 7)),
                                 reads=[actT.b, r.b], writes=[pbd.b])
                    K.op("act", lambda e, half=half, ys=ys, pbd=pbd: e.activation(out=ys[:, half * 512:(half + 1) * 512], in_=pbd[:], func=AF.Copy),
                         reads=[pbd.b], writes=[ys.b])
                K.dma("sp", lambda e, ys=ys, rb_=rb_: e.dma_start(out=Ys[ex_ * CAP + rb_ * 128:ex_ * CAP + (rb_ + 1) * 128, :], in_=ys[:]),
                      reads=[ys.b], writes=[bYs])

        issue_loads(NR)
        load_bias(0)
        prep(0)
        for ex_ in range(nexp):
            gate_up(ex_)
            if ex_ + 1 < nexp:
                load_bias(ex_ + 1)
                prep(ex_ + 1)
            down(ex_)
            issue_loads(4)
        K.barrier()
        esD2.close()

        esD3 = ExitStack()
        yk = [sb(esD3, "yk%d" % i, [128, D]) for i in range(4)]
        if partner:
            xo16 = [sb(esD3, "xo16%d" % i, [128, D], BF16) for i in range(2)]
        load_ln(ln2_g, ln2_b)
        gi = 0
        for j in range(nq):
            for k4 in range(4):
                y = yk[gi % 4]; gi += 1
                K.dma("pool", lambda e, y=y, j=j, k4=k4: e.indirect_dma_start(out=y[:], out_offset=None, in_=Ys,
                                                                             in_offset=bass.IndirectOffsetOnAxis(slots[:, j, k4:k4 + 1], 0)),
                      reads=[bYs, slots.b], writes=[y.b])
                K.op("dve", lambda e, y=y, j=j, k4=k4: e.scalar_tensor_tensor(out=xres[j][:], in0=y[:], scalar=gates[:, j, k4:k4 + 1], in1=xres[j][:],
                                                                             op0=ALU.mult, op1=ALU.add), reads=[y.b, gates.b, xres[j].b], writes=[xres[j].b])
            layer_norm(xres[j])
            if last:
                K.dma("sp", lambda e, j=j: e.dma_start(out=yout[j], in_=xres[j][:]), reads=[xres[j].b])
            elif partner:
                xo = xo16[j % 2]
                K.op("act", lambda e, xo=xo, j=j: e.activation(out=xo[:], in_=xres[j][:], func=AF.Copy), reads=[xres[j].b], writes=[xo.b])
                K.dma("sp", lambda e, xo=xo, j=j: e.dma_start(out=xo1[j * 128:(j + 1) * 128, :], in_=xo[:]), reads=[xo.b], writes=[bxo1])
        K.barrier()
        esD3.close()


    run_layer(0, partner=True, zero_slabs=True)
    run_layer(0)
    run_layer(1)
    K.barrier()
    K.emit()
    K.close()
    top.close()
    return nc


def rope_tables():
    pos = np.arange(4096, dtype=np.float32)
    inv = (np.float32(500000.0) ** (-np.arange(0, 16, 2, dtype=np.float32) / np.float32(16))).astype(np.float32)
    ang = (pos[:, None] * inv[None, :]).astype(np.float32)
    return np.cos(ang).astype(np.float32), np.sin(ang).astype(np.float32)


def core_inputs(x_b, p_b, c, weights):
    cos, sin = rope_tables()
    xt = x_b.reshape(32, 128, D)
    own = np.ascontiguousarray(xt[c::2]); oth = np.ascontiguousarray(xt[(1 - c)::2])
    cs = np.concatenate([np.broadcast_to(cos[:, None, :], (4096, 8, 8)).reshape(4096, 64),
                         np.broadcast_to(sin[:, None, :], (4096, 8, 8)).reshape(4096, 64)], axis=1).reshape(32, 128, 128)
    tri = np.where(np.arange(128)[None, :] <= np.arange(128)[:, None], 0.0, NEG).astype(np.float32)
    othm = np.full((128, 128), 0.0 if c == 1 else NEG, np.float32)
    hm = np.zeros((128, 2), np.float32); hm[:, c] = 1.0
    pd = np.zeros((128, 4, 16), np.float32)
    for g, win in enumerate((2, 4, 8, 16)):
        if c == 0:
            pd[:, g, :] = 1.0 / np.minimum(np.arange(1, 17, dtype=np.float32), float(win))
        else:
            pd[:, g, :] = 1.0 / win
    m = dict(weights)
    c_ = 1 - c
    othm_p = np.full((128, 128), 0.0 if c_ == 1 else NEG, np.float32)
    hm_p = np.zeros((128, 2), np.float32); hm_p[:, c_] = 1.0
    pd_p = np.zeros((128, 4, 16), np.float32)
    for g, win in enumerate((2, 4, 8, 16)):
        pd_p[:, g, :] = (1.0 / np.minimum(np.arange(1, 17, dtype=np.float32), float(win))) if c_ == 0 else (1.0 / win)
    m.update(poth0=np.ascontiguousarray(p_b.reshape(2, 32, 128, 256)[0, c_::2]),
             dmask_p=np.concatenate([tri, othm_p], axis=1), hmix_p=hm_p, pdiv0_p=pd_p.reshape(128, 64))
    m.update(xown=own, xoth=oth, pown=np.ascontiguousarray(p_b.reshape(2, 32, 128, 256)[:, c::2]),
             cs_own=np.ascontiguousarray(cs[c::2]), cs_oth=np.ascontiguousarray(cs[(1 - c)::2]),
             dmask=np.concatenate([tri, othm], axis=1), hmix=hm, pdiv0=pd.reshape(128, 64))
    return m


LAYER_WEIGHTS = ("w_in", "pool_w", "pool_scale", "w_up_attn", "w_up_pool", "w_out", "ln1_g", "ln1_b", "router_w", "router_b",
                 "exp_w_gu", "exp_b_gu", "exp_w_down", "exp_b_down", "ple_w_gate", "ple_w_proj", "ln2_g", "ln2_b")


def kernel(**inputs):
    x = np.asarray(inputs["x"], dtype=np.float32)
    p = np.asarray(inputs["p"], dtype=np.float32)
    nc = build()
    w = {}
    for k in LAYER_WEIGHTS:
        a = np.asarray(inputs[k], dtype=np.float32)
        if k in ("exp_w_gu", "exp_w_down"):
            for i in range(2):
                w["%s_%d" % (k, i)] = np.ascontiguousarray(a[i])
        else:
            w[k] = np.ascontiguousarray(a)
    w["ln0_g"] = np.asarray(inputs["ln0_g"], dtype=np.float32)
    w["ln0_b"] = np.asarray(inputs["ln0_b"], dtype=np.float32)
    maps = []
    for core in range(8):
        b, c = core // 2, core % 2
        maps.append(core_inputs(x[b], p[:, b], c, w))
    res = run_bass_kernel_spmd(nc, maps, core_ids=list(range(8)))
    out = np.empty_like(x)
    for core in range(8):
        b, c = core // 2, core % 2
        out[b].reshape(32, 128, D)[c::2] = np.asarray(res.results[core]["yout"]).reshape(NT, 128, D)
    return out
```
